# Optimizing a Trainium2 kernel written in Bass

```python
import math
import jax, jax.numpy as jnp
from jax import lax
import numpy as np

D_MODEL = 1024
BATCH = 8
SEQ = 4096
DEPTH = 1

N_META = 16
DN_HEADS = 4
DN_HEAD_DIM = 128
DN_WIDTH = DN_HEADS * DN_HEAD_DIM
DN_CONV = 4
DN_CHUNK = 64
SB_HEADS = 8
SB_HEAD_DIM = 64
SB_WIDTH = SB_HEADS * SB_HEAD_DIM
SB_BLOCK = 128
N_BRANCH = 2
IN_COLS = 4 * DN_WIDTH + 2 * DN_HEADS + 3 * SB_WIDTH + N_BRANCH * D_MODEL
SPLIT_POINTS = [3 * DN_WIDTH, 4 * DN_WIDTH, 4 * DN_WIDTH + DN_HEADS, 4 * DN_WIDTH + 2 * DN_HEADS,
                4 * DN_WIDTH + 2 * DN_HEADS + 3 * SB_WIDTH]
COL_DN_V0 = 2 * DN_WIDTH
COL_SB_V0 = 4 * DN_WIDTH + 2 * DN_HEADS + 2 * SB_WIDTH
N_GROUPS = 4
EXPERTS_PER_GROUP = 8
TOP_K_IN_GROUP = 2
EXPERT_FF = 256
DEEPNORM_ALPHA = (2.0 * DEPTH) ** 0.25
DEEPNORM_BETA = (8.0 * DEPTH) ** -0.25
LN_EPS = 1e-5
RMS_EPS = 1e-6

kernel_name = "hybrid_gdn_stickbreak_hiermoe_block"


def layer_norm(x, g, b):
    xf = x.astype(jnp.float32)
    mu = jnp.mean(xf, -1, keepdims=True)
    var = jnp.mean(jnp.square(xf - mu), -1, keepdims=True)
    return ((xf - mu) * lax.rsqrt(var + LN_EPS) * g.astype(jnp.float32) + b.astype(jnp.float32)).astype(x.dtype)


def rms_norm(x, g):
    xf = x.astype(jnp.float32)
    return xf * lax.rsqrt(jnp.mean(xf * xf, -1, keepdims=True) + RMS_EPS) * g.astype(jnp.float32)


def l2_normalize(x):
    xf = x.astype(jnp.float32)
    return xf * lax.rsqrt(jnp.sum(xf * xf, -1, keepdims=True) + RMS_EPS)


def split_heads(t, n):
    b, l, _ = t.shape
    return t.reshape(b, l, n, -1).transpose(0, 2, 1, 3)


def merge_heads(t):
    b, h, l, d = t.shape
    return t.transpose(0, 2, 1, 3).reshape(b, l, h * d)


def causal_depthwise_conv(x, w):
    c = x.shape[-1]
    return lax.conv_general_dilated(x, w[:, None, :], window_strides=(1,), padding=[(w.shape[0] - 1, 0)],
                                    dimension_numbers=("NWC", "WIO", "NWC"), feature_group_count=c)


def gated_delta_rule_chunked(q, k, v, beta, g):
    b, h, l, dk = q.shape
    dv = v.shape[-1]
    c = DN_CHUNK
    n = l // c
    q = q.reshape(b, h, n, c, dk)
    k = k.reshape(b, h, n, c, dk)
    v = v.reshape(b, h, n, c, dv)
    beta = beta.reshape(b, h, n, c)
    decay = jnp.cumsum(g.reshape(b, h, n, c), axis=-1)
    idx = jnp.arange(c)
    causal = idx[:, None] >= idx[None, :]
    strict = idx[:, None] > idx[None, :]
    diff = decay[..., :, None] - decay[..., None, :]
    lmask = jnp.where(causal, jnp.exp(jnp.where(causal, diff, 0.0)), 0.0)
    k_beta = k * beta[..., None]
    v_beta = v * beta[..., None]
    a = jnp.where(strict, jnp.einsum("bhnid,bhnjd->bhnij", k_beta, k) * lmask, 0.0)
    m = a + jnp.eye(c, dtype=a.dtype)
    u = lax.linalg.triangular_solve(m, v_beta, left_side=True, lower=True, unit_diagonal=True)
    w = lax.linalg.triangular_solve(m, k_beta * jnp.exp(decay)[..., None], left_side=True, lower=True,
                                    unit_diagonal=True)
    qk = jnp.where(causal, jnp.einsum("bhnid,bhnjd->bhnij", q, k) * lmask, 0.0)
    q_dec = q * jnp.exp(decay)[..., None]
    k_dec = k * jnp.exp(decay[..., -1:] - decay)[..., None]
    chunk_decay = jnp.exp(decay[..., -1])
    xs = tuple(jnp.moveaxis(t, 2, 0) for t in (q_dec, k_dec, u, w, qk, chunk_decay))

    def step(state, inp):
        qd, kd, uc, wc, qkc, cd = inp
        v_new = uc - jnp.einsum("bhcd,bhde->bhce", wc, state)
        o = jnp.einsum("bhcd,bhde->bhce", qd, state) + jnp.einsum("bhij,bhje->bhie", qkc, v_new)
        state = state * cd[..., None, None] + jnp.einsum("bhcd,bhce->bhde", kd, v_new)
        return state, o

    s0 = jnp.zeros((b, h, dk, dv), jnp.float32)
    _, o = lax.scan(step, s0, xs)
    return jnp.moveaxis(o, 0, 2).reshape(b, h, l, dv)


def stick_breaking_attention(q, k, v):
    l = q.shape[2]
    scale = q.shape[-1] ** -0.5
    bounds = [0] + list(range(N_META, l, SB_BLOCK)) + [l]
    outs = []
    for s0, s1 in zip(bounds[:-1], bounds[1:]):
        z = jnp.einsum("bhqd,bhkd->bhqk", q[:, :, s0:s1], k[:, :, :s1]).astype(jnp.float32) * scale
        mask = jnp.arange(s1)[None, :] < jnp.arange(s0, s1)[:, None]
        log_keep = jnp.where(mask, jax.nn.log_sigmoid(-z), 0.0)
        later = lax.cumsum(log_keep, axis=3, reverse=True) - log_keep
        weights = jnp.where(mask, jnp.exp(jax.nn.log_sigmoid(z) + later), 0.0)
        outs.append(jnp.einsum("bhqk,bhkd->bhqd", weights.astype(v.dtype), v[:, :, :s1]))
    return jnp.concatenate(outs, axis=2)


def hybrid_mixer(x, w_in, b_gate, dn_conv_w, dn_a_log, dn_dt_bias, dn_norm_g, w_branch_dn, w_branch_sb, w_out):
    b, l, d = x.shape
    proj = x @ w_in
    dn_qkv, dn_z, dn_b, dn_a, sb_qkv, gates = jnp.split(proj, SPLIT_POINTS, axis=-1)
    dn_qkv = jax.nn.silu(causal_depthwise_conv(dn_qkv, dn_conv_w))
    q, k, v = jnp.split(dn_qkv, 3, axis=-1)
    q = l2_normalize(split_heads(q, DN_HEADS)) * (DN_HEAD_DIM ** -0.5)
    k = l2_normalize(split_heads(k, DN_HEADS))
    v = split_heads(v, DN_HEADS).astype(jnp.float32)
    beta = jax.nn.sigmoid(dn_b.astype(jnp.float32)).transpose(0, 2, 1)
    g = (-jnp.exp(dn_a_log.astype(jnp.float32))
         * jax.nn.softplus(dn_a.astype(jnp.float32) + dn_dt_bias.astype(jnp.float32))).transpose(0, 2, 1)
    pad = (DN_CHUNK - N_META % DN_CHUNK) % DN_CHUNK
    front = lambda t: jnp.pad(t, [(0, 0), (0, 0), (pad, 0)] + [(0, 0)] * (t.ndim - 3))
    o_dn = gated_delta_rule_chunked(front(q), front(k), front(v), front(beta), front(g))[:, :, pad:]
    o_dn = merge_heads(rms_norm(o_dn, dn_norm_g)) * jax.nn.silu(dn_z.astype(jnp.float32))
    sq, sk, sv = (split_heads(t, SB_HEADS) for t in jnp.split(sb_qkv, 3, axis=-1))
    o_sb = merge_heads(stick_breaking_attention(sq, sk, sv))
    gate = jax.nn.sigmoid(gates.reshape(b, l, N_BRANCH, d) + b_gate)
    merged = (gate[:, :, 0] * (o_dn.astype(x.dtype) @ w_branch_dn)
              + gate[:, :, 1] * (o_sb.astype(x.dtype) @ w_branch_sb))
    return merged @ w_out


def hierarchical_moe(h, router_group_w, router_group_b, router_expert_w, router_expert_b,
                     expert_w_gate, expert_w_up, expert_w_down):
    b, l, d = h.shape
    t = h.reshape(b * l, d)
    group_probs = jax.nn.softmax((t @ router_group_w).astype(jnp.float32) + router_group_b, axis=-1)
    g_idx = jnp.argmax(group_probs, axis=-1)
    g_prob = jnp.max(group_probs, axis=-1)
    exp_logits = jnp.einsum("td,gde->tge", t, router_expert_w).astype(jnp.float32) + router_expert_b
    sel_logits = jnp.take_along_axis(exp_logits, g_idx[:, None, None], axis=1)[:, 0]
    top_logit, top_idx = lax.top_k(sel_logits, TOP_K_IN_GROUP)
    top_w = jax.nn.softmax(top_logit, axis=-1) * g_prob[:, None]
    combine_e = jnp.sum(jax.nn.one_hot(top_idx, EXPERTS_PER_GROUP) * top_w[..., None], axis=1)
    combine = jax.nn.one_hot(g_idx, N_GROUPS)[:, :, None] * combine_e[:, None, :]
    y = jnp.zeros_like(t)
    for gi in range(N_GROUPS):
        hid = (jax.nn.silu(jnp.einsum("td,edf->tef", t, expert_w_gate[gi]))
               * jnp.einsum("td,edf->tef", t, expert_w_up[gi]))
        hid = hid * combine[:, gi, :, None].astype(hid.dtype)
        y = y + jnp.einsum("tef,efd->td", hid, expert_w_down[gi])
    return y.reshape(b, l, d)


def setup_inputs(seed: int = 0) -> dict:
    key = jax.random.key(seed)
    ks = jax.random.split(key, 24)
    f32 = jnp.float32
    nrm = lambda k, shape, scale: jax.random.normal(k, shape, f32) * scale
    col_scale = np.ones((IN_COLS,), np.float32)
    col_scale[COL_DN_V0:COL_DN_V0 + DN_WIDTH] = DEEPNORM_BETA
    col_scale[COL_SB_V0:COL_SB_V0 + SB_WIDTH] = DEEPNORM_BETA
    dt = jnp.exp(jax.random.uniform(ks[6], (DEPTH, DN_HEADS), f32, math.log(1e-3), math.log(1e-1)))
    return {
        "x": nrm(ks[0], (BATCH, SEQ, D_MODEL), 1.0),
        "meta_tokens": nrm(ks[1], (N_META, D_MODEL), 1.0),
        "ln_emb_g": 1.0 + nrm(ks[2], (D_MODEL,), 0.05),
        "ln_emb_b": nrm(ks[3], (D_MODEL,), 0.02),
        "w_in": nrm(ks[4], (DEPTH, D_MODEL, IN_COLS), D_MODEL ** -0.5) * jnp.asarray(col_scale),
        "b_gate": nrm(ks[5], (DEPTH, N_BRANCH, D_MODEL), 0.02),
        "dn_conv_w": nrm(ks[7], (DEPTH, DN_CONV, 3 * DN_WIDTH), DN_CONV ** -0.5),
        "dn_a_log": jnp.log(jax.random.uniform(ks[8], (DEPTH, DN_HEADS), f32, 1.0, 16.0)),
        "dn_dt_bias": jnp.log(jnp.expm1(dt)),
        "dn_norm_g": 1.0 + nrm(ks[9], (DEPTH, DN_HEAD_DIM), 0.05),
        "w_branch_dn": nrm(ks[10], (DEPTH, DN_WIDTH, D_MODEL), DEEPNORM_BETA * DN_WIDTH ** -0.5),
        "w_branch_sb": nrm(ks[11], (DEPTH, SB_WIDTH, D_MODEL), DEEPNORM_BETA * SB_WIDTH ** -0.5),
        "w_out": nrm(ks[12], (DEPTH, D_MODEL, D_MODEL), DEEPNORM_BETA * D_MODEL ** -0.5),
        "ln1_g": 1.0 + nrm(ks[13], (DEPTH, D_MODEL), 0.05),
        "ln1_b": nrm(ks[14], (DEPTH, D_MODEL), 0.02),
        "router_group_w": nrm(ks[15], (DEPTH, D_MODEL, N_GROUPS), D_MODEL ** -0.5),
        "router_group_b": nrm(ks[16], (DEPTH, N_GROUPS), 0.01),
        "router_expert_w": nrm(ks[17], (DEPTH, N_GROUPS, D_MODEL, EXPERTS_PER_GROUP), D_MODEL ** -0.5),
        "router_expert_b": nrm(ks[18], (DEPTH, N_GROUPS, EXPERTS_PER_GROUP), 0.01),
        "expert_w_gate": nrm(ks[19], (DEPTH, N_GROUPS, EXPERTS_PER_GROUP, D_MODEL, EXPERT_FF), D_MODEL ** -0.5),
        "expert_w_up": nrm(ks[20], (DEPTH, N_GROUPS, EXPERTS_PER_GROUP, D_MODEL, EXPERT_FF),
                           DEEPNORM_BETA * D_MODEL ** -0.5),
        "expert_w_down": nrm(ks[21], (DEPTH, N_GROUPS, EXPERTS_PER_GROUP, EXPERT_FF, D_MODEL),
                             DEEPNORM_BETA * EXPERT_FF ** -0.5),
        "ln2_g": 1.0 + nrm(ks[22], (DEPTH, D_MODEL), 0.05),
        "ln2_b": nrm(ks[23], (DEPTH, D_MODEL), 0.02),
    }


def reference(x, meta_tokens, ln_emb_g, ln_emb_b, w_in, b_gate, dn_conv_w, dn_a_log, dn_dt_bias, dn_norm_g,
              w_branch_dn, w_branch_sb, w_out, ln1_g, ln1_b, router_group_w, router_group_b, router_expert_w,
              router_expert_b, expert_w_gate, expert_w_up, expert_w_down, ln2_g, ln2_b):
    b = x.shape[0]
    meta = jnp.broadcast_to(meta_tokens[None].astype(x.dtype), (b, N_META, x.shape[-1]))
    h = layer_norm(jnp.concatenate([meta, x], axis=1), ln_emb_g, ln_emb_b)
    for i in range(DEPTH):
        mix = hybrid_mixer(h, w_in[i], b_gate[i], dn_conv_w[i], dn_a_log[i], dn_dt_bias[i], dn_norm_g[i],
                           w_branch_dn[i], w_branch_sb[i], w_out[i])
        h = layer_norm(DEEPNORM_ALPHA * h + mix, ln1_g[i], ln1_b[i])
        ffn = hierarchical_moe(h, router_group_w[i], router_group_b[i], router_expert_w[i], router_expert_b[i],
                               expert_w_gate[i], expert_w_up[i], expert_w_down[i])
        h = layer_norm(DEEPNORM_ALPHA * h + ffn, ln2_g[i], ln2_b[i])
    return h[:, N_META:]
```

```python
import numpy as np
from contextlib import ExitStack
import concourse.bass as bass
import concourse.mybir as mybir
from concourse.bass_utils import run_bass_kernel_spmd

F32 = mybir.dt.float32
BF16 = mybir.dt.bfloat16
AF = mybir.ActivationFunctionType
ALU = mybir.AluOpType

D = 1024
SEQ = 4096
NMETA = 16
L = SEQ + NMETA
IN_COLS = 5640
C_DNQKV, C_DNZ, C_BA, C_SBQ, C_SBK, C_SBV, C_GATE = 0, 1536, 2048, 2056, 2568, 3080, 3592
ALPHA = 2.0 ** 0.25
LN_EPS = 1e-5
RMS_EPS = 1e-6
NG, NE, FF = 4, 8, 256


class Region:
    __slots__ = ("name", "last_write", "readers", "psum", "t")

    def __init__(self, name="", psum=False):
        self.name = name
        self.last_write = None
        self.readers = []
        self.psum = psum
        self.t = 0.0


class Op:
    __slots__ = ("eng", "fn", "reads", "writes", "dma", "signal", "sem", "val", "waits", "idx", "cost")


class T:
    def __init__(self, t, name):
        self.t = t
        self.r = Region(name)

    def __getitem__(self, idx):
        return self.t[idx]


def _reg(x):
    return x.r if isinstance(x, T) else x


class Prog:
    ENGS = ("pe", "act", "dve", "pool", "sp")

    def __init__(self, nc, n_dma_sems=20):
        self.nc = nc
        self.ops = []
        self.n_dma_sems = n_dma_sems
        self.bar_regions = {e: Region("bar_" + e) for e in self.ENGS}
        self.live = {}

    def op(self, eng, fn, reads=(), writes=(), dma=False, cost=0.3):
        o = Op()
        o.cost = cost
        o.eng = eng
        o.fn = fn
        o.reads = tuple(_reg(r) for r in reads)
        o.writes = tuple(_reg(w) for w in writes)
        for r in o.reads + o.writes:
            self.live[id(r)] = r
        o.dma = dma
        o.signal = False
        o.sem = None
        o.val = 0
        o.waits = []
        o.idx = len(self.ops)
        self.ops.append(o)
        return o

    def barrier(self):
        live = list(self.live.values())
        self.op("sp", lambda e: e.nop(), reads=live, writes=live + [self.bar_regions["sp"]])
        for e in ("pe", "act", "dve", "pool"):
            self.op(e, lambda h: h.nop(), reads=[self.bar_regions["sp"]], writes=[self.bar_regions[e]])
        self.live = {}

    @staticmethod
    def _n(ap):
        n = 1
        for d in ap.shape[1:]:
            n *= int(d)
        return n

    def _ecost(self, eng, ap):
        n = self._n(ap)
        if eng == "act":
            return 0.13 + n / 1400.0
        if eng == "pool":
            return 0.15 + n * 0.0022
        return 0.08 + n / 960.0

    def dma(self, out, in_, reads=(), writes=(), eng="sp", **kw):
        return self.op(eng, lambda e: e.dma_start(out=out, in_=in_, **kw), reads, writes, dma=True, cost=2.0)

    def mm(self, out, lhsT, rhs, start=True, stop=True, reads=(), writes=()):
        return self.op("pe", lambda e: e.matmul(out, lhsT, rhs, start=start, stop=stop), reads, writes,
                       cost=0.05 + self._n(out) * 0.00056)

    def tr(self, out, in_, ident, reads=(), writes=()):
        return self.op("pe", lambda e: e.transpose(out, in_, ident), reads, writes, cost=0.15)

    def act(self, out, in_, func, reads=(), writes=(), **kw):
        return self.op("act", lambda e: e.activation(out=out, in_=in_, func=func, **kw), reads, writes,
                       cost=self._ecost("act", out))

    def tt(self, eng, out, in0, in1, op, reads=(), writes=()):
        return self.op(eng, lambda e: e.tensor_tensor(out=out, in0=in0, in1=in1, op=op), reads, writes,
                       cost=self._ecost(eng, out))

    def ts(self, eng, out, in0, s1, s2, op0, op1=None, reads=(), writes=()):
        if op1 is None:
            return self.op(eng, lambda e: e.tensor_scalar(out=out, in0=in0, scalar1=s1, scalar2=None, op0=op0),
                           reads, writes, cost=self._ecost(eng, out))
        return self.op(eng, lambda e: e.tensor_scalar(out=out, in0=in0, scalar1=s1, scalar2=s2, op0=op0, op1=op1),
                       reads, writes, cost=self._ecost(eng, out))

    def stt(self, out, in0, scalar, in1, op0, op1, reads=(), writes=()):
        return self.op("dve", lambda e: e.scalar_tensor_tensor(out=out, in0=in0, scalar=scalar, in1=in1,
                                                                op0=op0, op1=op1), reads, writes,
                       cost=self._ecost("dve", out))

    def copy(self, eng, out, in_, reads=(), writes=()):
        if eng == "act":
            return self.op("act", lambda e: e.copy(out=out, in_=in_), reads, writes, cost=self._ecost("act", out))
        return self.op(eng, lambda e: e.tensor_copy(out=out, in_=in_), reads, writes, cost=self._ecost(eng, out))

    def sim_new_ops(self, n0):
        if not hasattr(self, "eng_t"):
            self.eng_t = {e: 0.0 for e in self.ENGS}
        fin_max = 0.0
        for o in self.ops[n0:]:
            start = self.eng_t[o.eng]
            for r in o.reads + o.writes:
                if r.t + 0.15 > start:
                    start = r.t + 0.15
            fin = start + o.cost
            if not o.dma:
                self.eng_t[o.eng] = fin
            else:
                self.eng_t[o.eng] = start + 0.05
            for r in o.writes:
                r.t = fin
            for r in o.reads:
                if o.dma or r.psum:
                    r.t = max(r.t, fin)
            fin_max = max(fin_max, fin)
        return fin_max

    def run_sched(self, queue, slotsets=None, prestarted=()):
        slotsets = slotsets or {}
        free = {k: list(range(len(v))) for k, v in slotsets.items()}
        active = [[g, None, None, 0.0, None] for g in prestarted]
        queue = list(queue)
        done = set()

        def startable(it):
            if it == "drain":
                return False
            if it[0] is not None and not free[it[0]]:
                return False
            return all(a in done for a in (it[3] if len(it) > 3 else ()))

        while queue or active:
            while queue and startable(queue[0]):
                it = queue.pop(0)
                cls, mk = it[0], it[1]
                name = it[2] if len(it) > 2 else None
                t_now = min([a[3] for a in active], default=0.0)
                if cls is None:
                    active.append([mk(None), None, None, t_now, name])
                else:
                    si = free[cls].pop(0)
                    active.append([mk(slotsets[cls][si]), cls, si, t_now, name])
            if queue and queue[0] == "drain" and not active:
                queue.pop(0)
                continue
            assert active, "scheduler deadlock: head of queue waits for a generator that was never started"
            item = min(active, key=lambda a: a[3])
            n0 = len(self.ops)
            try:
                next(item[0])
                fin = self.sim_new_ops(n0)
                item[3] = max(item[3], fin) if fin > 0 else item[3] + 0.01
            except StopIteration:
                self.sim_new_ops(n0)
                active.remove(item)
                if item[1] is not None:
                    free[item[1]].append(item[2])
                if item[4] is not None:
                    done.add(item[4])


    def finalize(self, stack):
        nc = self.nc
        ops = self.ops
        deps_of = []
        for o in ops:
            deps = {}
            for r in o.reads:
                if r.last_write is not None:
                    deps[r.last_write] = "raw"
                if r.psum:
                    for rd in r.readers:
                        if ops[rd].eng != o.eng and rd not in deps:
                            deps[rd] = "rr"
            for w in o.writes:
                if w.last_write is not None and w.last_write not in deps:
                    deps[w.last_write] = "waw"
                for rd in w.readers:
                    if rd not in deps:
                        deps[rd] = "war"
            for r in o.reads:
                if o.dma:
                    r.readers.append(o.idx)
                else:
                    r.readers = [q for q in r.readers if ops[q].dma or ops[q].eng != o.eng]
                    r.readers.append(o.idx)
            for w in o.writes:
                w.last_write = o.idx
                w.readers = []
            keep = []
            for j, kind in deps.items():
                if j == o.idx:
                    continue
                p = ops[j]
                if p.eng == o.eng and not p.dma and not o.dma:
                    if o.eng == "pe" or kind == "rr":
                        continue
                keep.append(j)
            deps_of.append(keep)
            for j in keep:
                ops[j].signal = True
        eng_sem = {e: stack.enter_context(nc.semaphore("s_" + e)) for e in self.ENGS}
        dma_engs = ("sp", "pool", "act")
        dma_pool = {e: [stack.enter_context(nc.semaphore("d_%s%d" % (e, i))) for i in range(self.n_dma_sems)]
                    for e in dma_engs}
        dma_val = {e: [0] * self.n_dma_sems for e in dma_engs}
        dma_rr = {e: 0 for e in dma_engs}
        eng_cnt = {e: 0 for e in self.ENGS}
        for o in ops:
            pre = []
            if o.dma:
                k = dma_rr[o.eng]
                dma_rr[o.eng] = (k + 1) % self.n_dma_sems
                if dma_val[o.eng][k] > 0:
                    pre.append((dma_pool[o.eng][k], dma_val[o.eng][k]))
                dma_val[o.eng][k] += 16
                o.sem = dma_pool[o.eng][k]
                o.val = dma_val[o.eng][k]
                o.signal = True
            elif o.signal:
                eng_cnt[o.eng] += 1
                o.sem = eng_sem[o.eng]
                o.val = eng_cnt[o.eng]
            best = {}
            for (sem, val) in pre + [(ops[j].sem, ops[j].val) for j in deps_of[o.idx]]:
                key = id(sem)
                if key not in best or best[key][1] < val:
                    best[key] = (sem, val)
            o.waits = list(best.values())
        self.stats = {e: sum(1 for o in ops if o.eng == e) for e in self.ENGS}
        self.stats["signals"] = dict(eng_cnt)
        per_eng = {e: [o for o in ops if o.eng == e] for e in self.ENGS}
        block = stack.enter_context(nc.Block())
        nds = self.n_dma_sems

        def emit_stream(e, handle):
            known = {}
            nw = 0
            for o in per_eng[e]:
                for (sem, val) in o.waits:
                    key = id(sem)
                    if known.get(key, 0) >= val:
                        continue
                    known[key] = val
                    handle.wait_ge(sem, val)
                    nw += 1
                ins = o.fn(handle)
                if o.signal:
                    ins.then_inc(o.sem, 16 if o.dma else 1)
            if e in dma_pool:
                for k in range(nds):
                    if dma_val[e][k] > 0 and known.get(id(dma_pool[e][k]), 0) < dma_val[e][k]:
                        handle.wait_ge(dma_pool[e][k], dma_val[e][k])
            self.stats["waits_" + e] = nw

        @block.tensor
        def _(h):
            emit_stream("pe", h)

        @block.scalar
        def _(h):
            emit_stream("act", h)

        @block.vector
        def _(h):
            emit_stream("dve", h)

        @block.gpsimd
        def _(h):
            emit_stream("pool", h)

        @block.sync
        def _(h):
            emit_stream("sp", h)


def rsqrt(P, out, in_, eps, reads, wt):
    P.act(out, in_, AF.Ln, reads=reads, writes=[wt], bias=eps)
    P.act(out, out, AF.Exp, reads=[wt], writes=[wt], scale=-0.5)


class Rot:
    def __init__(self, items):
        self.items = items
        self.i = 0

    def next(self):
        x = self.items[self.i % len(self.items)]
        self.i += 1
        return x


def st_range(j):
    if j == 0:
        return 0, NMETA
    return NMETA + 512 * (j - 1), 512


N_ST = 9


class Ctx:
    pass


def build(debug=(), phases=(1, 2, 3, 4, 5), scratch_in=()):
    nc = bass.Bass("TRN2", target_bir_lowering=False)
    c = Ctx()
    c.nc = nc
    c.debug = debug
    c.phases = phases

    def din(name, shape):
        return nc.dram_tensor(name, list(shape), F32, kind="ExternalInput").ap()

    c.x = din("x", (SEQ, D))
    c.meta = din("meta_tokens", (NMETA, D))
    c.ln_emb_g = din("ln_emb_g", (D,))
    c.ln_emb_b = din("ln_emb_b", (D,))
    c.w_in = din("w_in", (D, IN_COLS))
    c.b_gate = din("b_gate", (2, D))
    c.conv_w = din("dn_conv_w", (4, 1536))
    c.a_log = din("dn_a_log", (4,))
    c.dt_bias = din("dn_dt_bias", (4,))
    c.norm_g = din("dn_norm_g", (128,))
    c.w_bdn = din("w_branch_dn", (512, D))
    c.w_bsb = din("w_branch_sb", (512, D))
    c.w_out = din("w_out", (D, D))
    c.ln1_g = din("ln1_g", (D,))
    c.ln1_b = din("ln1_b", (D,))
    c.rg_w = din("router_group_w", (D, NG))
    c.rg_b = din("router_group_b", (NG,))
    c.re_w = din("router_expert_w", (NG, D, NE))
    c.re_b = din("router_expert_b", (NG, NE))
    c.e_gate = din("expert_w_gate", (NG * NE, D, FF))
    c.e_up = din("expert_w_up", (NG * NE, D, FF))
    c.e_down = din("expert_w_down", (NG * NE, FF, D))
    c.ln2_g = din("ln2_g", (D,))
    c.ln2_b = din("ln2_b", (D,))
    c.out = nc.dram_tensor("out", [SEQ, D], F32, kind="ExternalOutput").ap()

    def scratch(name, shape, dt):
        kind = "ExternalOutput" if name in debug else ("ExternalInput" if name in scratch_in else "Internal")
        return T(nc.dram_tensor(name, list(shape), dt, kind=kind).ap(), name)

    c.h0s = scratch("h0s", (L, D), F32)
    c.qT_dn = scratch("qT_dn", (4, 128, L), BF16)
    c.kT_dn = scratch("kT_dn", (4, 128, L), BF16)
    c.k_dn = scratch("k_dn", (L, 512), BF16)
    c.v_dn = scratch("v_dn", (L, 512), BF16)
    c.bgs = scratch("bgs", (L, 8), F32)
    c.zsT = scratch("zsT", (4, 128, L), BF16)
    c.qT_sb = scratch("qT_sb", (4, 128, L), BF16)
    c.kT_sb = scratch("kT_sb", (4, 128, L), BF16)
    c.v_sb = scratch("v_sb", (L, 512), BF16)
    c.gsT = scratch("gsT", (16, 128, L), BF16)
    c.odnT = scratch("odnT", (4, 128, L), BF16)
    c.osbT = scratch("osbT", (4, 128, L), BF16)
    c.h1s = scratch("h1s", (SEQ, D), F32)
    c.h1T = scratch("h1T", (8, 128, SEQ), BF16)
    c.combs = scratch("combs", (SEQ, 32), F32)

    with ExitStack() as st:
        P = Prog(nc)
        c.P = P
        c.st = st
        phase_consts(c)
        if 1 in c.phases:
            phase1(c)
        if 2 in c.phases:
            phase2(c)
        if 3 in c.phases:
            phase3(c)
        if 4 in c.phases:
            phase4(c)
        if 5 in c.phases:
            phase5(c)
        P.finalize(st)
        c.stats = P.stats
    return nc, c


def sb(c, st, name, shape, dt):
    return T(st.enter_context(c.nc.sbuf_tensor(name, list(shape), dt)), name)


def ps(c, st, name, shape, dt=F32):
    nbytes = int(np.prod(shape[1:])) * (4 if dt == F32 else 2)
    assert nbytes % 2048 == 0, (name, shape)
    t = T(st.enter_context(c.nc.psum_tensor(name, list(shape), dt)), name)
    t.r.psum = True
    return t


def phase_consts(c):
    P, st, nc = c.P, c.st, c.nc
    c.ident = sb(c, st, "ident", (128, 128), F32)
    c.identb = sb(c, st, "identb", (128, 128), BF16)
    c.onesb = sb(c, st, "onesb", (128, 128), BF16)
    P.op("pool", lambda e: e.memset(c.ident[:], 0.0), writes=[c.ident])
    P.op("pool", lambda e: e.affine_select(out=c.ident[:], in_=c.ident[:], pattern=[[-1, 128]],
                                           compare_op=ALU.not_equal, fill=1.0, base=0, channel_multiplier=1),
         reads=[c.ident], writes=[c.ident])
    P.copy("pool", c.identb[:], c.ident[:], reads=[c.ident], writes=[c.identb])
    P.op("pool", lambda e: e.memset(c.onesb[:], 1.0), writes=[c.onesb])
    c.ident4 = sb(c, st, "ident4", (128, 4, 128), F32)
    for h in range(4):
        P.copy("pool", c.ident4[:, h, :], c.ident[:], reads=[c.ident], writes=[c.ident4])


def run_window(P, queue, slots, G):
    free = list(range(len(slots)))
    active = []
    queue = list(queue)
    while queue or active:
        while queue and queue[0] != "drain" and len(active) < G and free:
            mk = queue.pop(0)
            si = free.pop(0)
            active.append((mk(slots[si]), si))
        if queue and queue[0] == "drain" and not active:
            queue.pop(0)
            continue
        for item in list(active):
            try:
                next(item[0])
            except StopIteration:
                active.remove(item)
                free.append(item[1])


def phase1(c):
    P, nc = c.P, c.nc
    NSLOT = 7
    with ExitStack() as st:
        w_in = sb(c, st, "w_in_sb", (128, 8, IN_COLS), BF16)
        gbc = sb(c, st, "gbc", (128, D), F32)
        bbc = sb(c, st, "bbc", (128, D), F32)
        cw = sb(c, st, "cw", (128, 12, 4), F32)
        cwraw = sb(c, st, "cwraw", (4, 1536), F32)
        bgT = sb(c, st, "bgT", (128, 16), F32)
        bgraw = sb(c, st, "bgraw", (16, 128), F32)
        alog = sb(c, st, "alog", (128, 4), F32)
        negA = sb(c, st, "negA", (128, 4), F32)
        dtb = sb(c, st, "dtb", (128, 4), F32)
        hist = sb(c, st, "hist", (128, 12, 3), F32)
        h0Ts = [sb(c, st, "h0T%d" % i, (128, 8, 512), BF16) for i in range(2)]
        xb = Rot([sb(c, st, "xb%d" % i, (128, D), F32) for i in range(4)])
        hb = Rot([sb(c, st, "hb%d" % i, (128, D), F32) for i in range(4)])
        stats = Rot([sb(c, st, "stats%d" % i, (128, 16), F32) for i in range(4)])
        lnslots = [{} for i in range(4)]
        slots = []
        for i in range(NSLOT):
            slots.append({"pc": sb(c, st, "pc%d" % i, (128, 515), F32), "acc": sb(c, st, "acc%d" % i, (128, 512), F32),
                          "sq": sb(c, st, "sq%d" % i, (128, 512), BF16), "o": sb(c, st, "o%d" % i, (128, 512), BF16),
                          "tk": sb(c, st, "tk%d" % i, (128, 512), BF16), "bg": sb(c, st, "bgb%d" % i, (128, 16), F32)})
        pT = ps(c, st, "pT", (128, D), F32)
        pmm = Rot([ps(c, st, "pmm%d" % i, (128, 512), F32) for i in range(4)])
        ptr = Rot([ps(c, st, "ptr%d" % i, (128, 1024), BF16) for i in range(2)])

        CG = ((0, 1536), (1536, 3080), (3080, 4360), (4360, IN_COLS))
        w_in_rr = [[Region("w_in_%d_%d" % (k, g)) for g in range(4)] for k in range(8)]
        for g, (c0, c1) in enumerate(CG):
            for k in range(8):
                P.dma(w_in[:, k, c0:c1], c.w_in[k * 128:(k + 1) * 128, c0:c1], writes=[w_in_rr[k][g]], eng="pool")

        def wreg(k, c0, width=128):
            return [w_in_rr[k][g] for g, (a0, a1) in enumerate(CG) if c0 < a1 and c0 + width > a0]
        P.dma(gbc[:], c.ln_emb_g.partition_broadcast(128), writes=[gbc])
        P.dma(bbc[:], c.ln_emb_b.partition_broadcast(128), writes=[bbc])
        P.dma(cwraw[:], c.conv_w, writes=[cwraw])
        P.dma(bgraw[:], c.b_gate.rearrange("a (c p) -> (a c) p", p=128), writes=[bgraw])
        P.dma(alog[:], c.a_log.partition_broadcast(128), writes=[alog])
        P.dma(dtb[:], c.dt_bias.partition_broadcast(128), writes=[dtb])
        P.op("pool", lambda e: e.memset(hist[:], 0.0), writes=[hist])
        for cc in range(12):
            pp = pmm.next()
            P.tr(pp[:, 0:4], cwraw[0:4, cc * 128:(cc + 1) * 128], c.ident[0:4, 0:4], reads=[cwraw, c.ident],
                 writes=[pp])
            P.copy("dve", cw[:, cc, :], pp[:, 0:4], reads=[pp], writes=[cw])
        pp = pmm.next()
        P.tr(pp[:, 0:16], bgraw[0:16, :], c.ident[0:16, 0:16], reads=[bgraw, c.ident], writes=[pp])
        P.copy("dve", bgT[:], pp[:, 0:16], reads=[pp], writes=[bgT])
        P.act(negA[:], alog[:], AF.Exp, reads=[alog], writes=[negA])
        P.ts("dve", negA[:], negA[:], -1.0, None, ALU.mult, reads=[negA], writes=[negA])

        def gen_ln(j, s, slot):
            t0, Tn = st_range(j)
            h0T = h0Ts[j % 2]
            rows = min(128, Tn)
            r0 = t0 + s * 128
            xt, ht, stt_ = xb.next(), hb.next(), stats.next()
            if j == 0:
                P.dma(xt[:rows, :], c.meta[:, :], writes=[xt])
            else:
                P.dma(xt[:rows, :], c.x[r0 - NMETA:r0 - NMETA + rows, :], writes=[xt])
            yield
            P.op("dve", lambda e: e.bn_stats(out=stt_[:rows, 0:6], in_=xt[:rows, 0:512]), reads=[xt], writes=[stt_],
                 cost=0.7)
            P.op("dve", lambda e: e.bn_stats(out=stt_[:rows, 6:12], in_=xt[:rows, 512:1024]), reads=[xt],
                 writes=[stt_], cost=0.7)
            P.op("dve", lambda e: e.bn_aggr(out=stt_[:rows, 12:14], in_=stt_[:rows, 0:12]), reads=[stt_],
                 writes=[stt_])
            P.act(stt_[:rows, 14:15], stt_[:rows, 13:14], AF.Ln, reads=[stt_], writes=[stt_], bias=LN_EPS)
            yield
            P.act(stt_[:rows, 14:15], stt_[:rows, 14:15], AF.Exp, reads=[stt_], writes=[stt_], scale=-0.5)
            P.stt(stt_[:rows, 15:16], stt_[:rows, 12:13], -1.0, stt_[:rows, 14:15], ALU.mult, ALU.mult, reads=[stt_],
                  writes=[stt_])
            yield
            P.act(xt[:rows, :], xt[:rows, :], AF.Identity, reads=[xt, stt_], writes=[xt], scale=stt_[:rows, 14:15],
                  bias=stt_[:rows, 15:16])
            yield
            P.tt("dve", xt[:rows, :], xt[:rows, :], gbc[:rows, :], ALU.mult, reads=[xt, gbc], writes=[xt])
            yield
            P.tt("pool", ht[:rows, :], xt[:rows, :], bbc[:rows, :], ALU.add, reads=[xt, bbc], writes=[ht])
            P.dma(c.h0s[r0:r0 + rows, :], ht[:rows, :], reads=[ht], writes=[Region()])
            yield
            for k in range(8):
                P.tr(pT[:, k * 128:k * 128 + rows], ht[:rows, k * 128:(k + 1) * 128], c.ident[:rows, :rows],
                     reads=[ht, c.ident], writes=[pT])
            P.copy("act", h0T[:, :, s * 128:s * 128 + rows],
                   pT[:].rearrange("p (k t) -> p k t", k=8)[:, :, 0:rows], reads=[pT], writes=[h0T])
            yield

        def proj(c0, j):
            t0, Tn = st_range(j)
            h0T = h0Ts[j % 2]
            pp = pmm.next()
            for k in range(8):
                P.mm(pp[:, :Tn], w_in[:, k, c0:c0 + 128], h0T[:, k, :Tn], start=(k == 0), stop=(k == 7),
                     reads=wreg(k, c0) + [h0T], writes=[pp])
            return pp

        def to_tok(j, o, tk, dst, h):
            t0, Tn = st_range(j)
            nsub = max(1, Tn // 128)
            rows = min(128, Tn)
            pt = ptr.next()
            for s in range(nsub):
                P.tr(pt[:rows, s * 128:(s + 1) * 128], o[:, s * 128:s * 128 + rows], c.identb[:, :],
                     reads=[o, c.identb], writes=[pt])
            P.copy("dve", tk[:rows, 0:nsub * 128], pt[:rows, 0:nsub * 128], reads=[pt], writes=[tk])
            for s in range(nsub):
                P.dma(dst[t0 + s * 128:t0 + s * 128 + rows, h * 128:(h + 1) * 128], tk[:rows, s * 128:(s + 1) * 128],
                      reads=[tk], writes=[Region()])

        def gen_dn(j, cc, slot):
            t0, Tn = st_range(j)
            kind, h = cc // 4, cc % 4
            pc, acc, sq, o, tk = slot["pc"], slot["acc"], slot["sq"], slot["o"], slot["tk"]
            pp = proj(C_DNQKV + cc * 128, j)
            P.copy("act", pc[:, 3:3 + Tn], pp[:, :Tn], reads=[pp], writes=[pc])
            P.copy("pool", pc[:, 0:3], hist[:, cc, :], reads=[hist], writes=[pc])
            yield
            P.ts("dve", acc[:, :Tn], pc[:, 3:3 + Tn], cw[:, cc, 3:4], None, ALU.mult, reads=[pc, cw], writes=[acc])
            for tap in (2, 1, 0):
                P.stt(acc[:, :Tn], pc[:, tap:tap + Tn], cw[:, cc, tap:tap + 1], acc[:, :Tn], ALU.mult, ALU.add,
                      reads=[pc, cw, acc], writes=[acc])
            P.copy("pool", hist[:, cc, :], pc[:, Tn:Tn + 3], reads=[pc], writes=[hist])
            yield
            if kind == 2:
                P.act(o[:, :Tn], acc[:, :Tn], AF.Silu, reads=[acc], writes=[o])
                yield
                to_tok(j, o, tk, c.v_dn, h)
                return
            sl = pc
            P.act(sl[:, 3:3 + Tn], acc[:, :Tn], AF.Silu, reads=[acc], writes=[pc])
            yield
            P.tt("dve", sq[:, :Tn], sl[:, 3:3 + Tn], sl[:, 3:3 + Tn], ALU.mult, reads=[pc], writes=[sq])
            yield
            p2 = pmm.next()
            P.mm(p2[:, :Tn], c.onesb[:, :], sq[:, :Tn], reads=[c.onesb, sq], writes=[p2])
            P.act(acc[:, :Tn], p2[:, :Tn], AF.Ln, reads=[p2], writes=[acc], bias=RMS_EPS)
            yield
            P.act(acc[:, :Tn], acc[:, :Tn], AF.Exp, reads=[acc], writes=[acc], scale=-0.5)
            if kind == 0:
                P.stt(o[:, :Tn], sl[:, 3:3 + Tn], 128.0 ** -0.5, acc[:, :Tn], ALU.mult, ALU.mult,
                      reads=[pc, acc], writes=[o])
                P.dma(c.qT_dn[h, :, t0:t0 + Tn], o[:, :Tn], reads=[o], writes=[Region()])
            else:
                P.tt("dve", o[:, :Tn], sl[:, 3:3 + Tn], acc[:, :Tn], ALU.mult, reads=[pc, acc], writes=[o])
                P.dma(c.kT_dn[h, :, t0:t0 + Tn], o[:, :Tn], reads=[o], writes=[Region()])
                yield
                to_tok(j, o, tk, c.k_dn, h)

        def gen_simple(j, kind, cc, slot):
            t0, Tn = st_range(j)
            o = slot["o"]
            if kind == "z":
                pp = proj(C_DNZ + cc * 128, j)
                P.act(o[:, :Tn], pp[:, :Tn], AF.Silu, reads=[pp], writes=[o])
                P.dma(c.zsT[cc, :, t0:t0 + Tn], o[:, :Tn], reads=[o], writes=[Region()])
            elif kind == "sbq":
                pp = proj(C_SBQ + cc * 128, j)
                P.ts("dve", o[:, :Tn], pp[:, :Tn], 0.125, None, ALU.mult, reads=[pp], writes=[o])
                P.dma(c.qT_sb[cc, :, t0:t0 + Tn], o[:, :Tn], reads=[o], writes=[Region()])
            elif kind == "sbk":
                pp = proj(C_SBK + cc * 128, j)
                P.copy("dve", o[:, :Tn], pp[:, :Tn], reads=[pp], writes=[o])
                P.dma(c.kT_sb[cc, :, t0:t0 + Tn], o[:, :Tn], reads=[o], writes=[Region()])
            else:
                pp = proj(C_GATE + cc * 128, j)
                P.act(o[:, :Tn], pp[:, :Tn], AF.Sigmoid, reads=[pp, bgT], writes=[o], bias=bgT[:, cc:cc + 1])
                P.dma(c.gsT[cc, :, t0:t0 + Tn], o[:, :Tn], reads=[o], writes=[Region()])
            yield

        def gen_tok(j, s, slot):
            t0, Tn = st_range(j)
            h0T = h0Ts[j % 2]
            rows = min(128, Tn)
            r0 = t0 + s * 128
            tk, bg = slot["tk"], slot["bg"]
            pp = pmm.next()
            for k in range(8):
                P.mm(pp[:rows, 0:512], h0T[:, k, s * 128:s * 128 + rows], w_in[:, k, C_SBV:C_SBV + 512],
                     start=(k == 0), stop=(k == 7), reads=wreg(k, C_SBV, 512) + [h0T], writes=[pp])
            P.copy("act", tk[:rows, :], pp[:rows, :], reads=[pp], writes=[tk])
            P.dma(c.v_sb[r0:r0 + rows, :], tk[:rows, :], reads=[tk], writes=[Region()])
            yield
            pp = pmm.next()
            for k in range(8):
                P.mm(pp[:rows, 0:8], h0T[:, k, s * 128:s * 128 + rows], w_in[:, k, C_BA:C_BA + 8],
                     start=(k == 0), stop=(k == 7), reads=wreg(k, C_BA, 8) + [h0T], writes=[pp])
            P.act(bg[:rows, 0:4], pp[:rows, 0:4], AF.Sigmoid, reads=[pp], writes=[bg])
            P.tt("dve", bg[:rows, 8:12], pp[:rows, 4:8], dtb[:rows, :], ALU.add, reads=[pp, dtb], writes=[bg])
            yield
            P.act(bg[:rows, 8:12], bg[:rows, 8:12], AF.Exp, reads=[bg], writes=[bg])
            P.act(bg[:rows, 8:12], bg[:rows, 8:12], AF.Ln, reads=[bg], writes=[bg], bias=1.0)
            P.tt("dve", bg[:rows, 4:8], bg[:rows, 8:12], negA[:rows, :], ALU.mult, reads=[bg, negA], writes=[bg])
            P.dma(c.bgs[r0:r0 + rows, :], bg[:rows, 0:8], reads=[bg], writes=[Region()])
            yield

        def nsub_of(j):
            return max(1, st_range(j)[1] // 128)

        def ln_items(j, after):
            return [("ln", (lambda sl_, j=j, s_=s_: gen_ln(j, s_, sl_)), "ln%d_%d" % (j, s_), tuple(after))
                    for s_ in range(nsub_of(j))]

        queue = ln_items(0, ())
        names = {}
        for j in range(N_ST):
            nsub = nsub_of(j)
            items = []
            names[j] = []

            def add(mk, name, after, j=j):
                items.append(("s", mk, name, tuple(after)))
                names[j].append(name)

            base = ["ln%d_%d" % (j, s_) for s_ in range(nsub)]
            for cc in range(12):
                add((lambda sl_, j=j, cc=cc: gen_dn(j, cc, sl_)), "dn%d_%d" % (j, cc),
                    base + (["dn%d_%d" % (j - 1, cc)] if j > 0 else []))
                if cc == 3 and j + 1 < N_ST:
                    items.extend(ln_items(j + 1, names[j - 1] if j > 0 else ()))
            for cc in range(4):
                add((lambda sl_, j=j, cc=cc: gen_simple(j, "z", cc, sl_)), "z%d_%d" % (j, cc), base)
            for s_ in range(nsub):
                add((lambda sl_, j=j, s_=s_: gen_tok(j, s_, sl_)), "tok%d_%d" % (j, s_), base)
            for cc in range(4):
                add((lambda sl_, j=j, cc=cc: gen_simple(j, "sbq", cc, sl_)), "sbq%d_%d" % (j, cc), base)
                add((lambda sl_, j=j, cc=cc: gen_simple(j, "sbk", cc, sl_)), "sbk%d_%d" % (j, cc), base)
            for cc in range(16):
                add((lambda sl_, j=j, cc=cc: gen_simple(j, "gate", cc, sl_)), "gate%d_%d" % (j, cc), base)
            queue += items
        P.run_sched(queue, {"s": slots, "ln": lnslots})
        P.barrier()


def chunk_range(ci):
    if ci == 0:
        return 0, NMETA
    return NMETA + 128 * (ci - 1), 128


N_CH = 33


def phase2(c):
    P, nc = c.P, c.nc
    GRP = 3
    NSLOT = 2 * GRP
    with ExitStack() as st:
        def t32(name):
            return sb(c, st, name, (128, 4, 128), F32)

        def t16(name):
            return sb(c, st, name, (128, 4, 128), BF16)

        Mst, Min, M32, M64, Mc32, Mc64 = t32("Mst"), t32("Min"), t32("M32"), t32("M64"), t32("Mc32"), t32("Mc64")
        ones32 = sb(c, st, "ones32", (128, 128), F32)
        normg = sb(c, st, "normg", (128, 1), F32)
        S = t32("S")
        Sb = t16("Sb")
        slots = []
        inner = []
        for g in range(GRP):
            d = {}
            for n in ("N", "NT", "Nd0", "NdT0", "Nd1", "NdT1", "C32", "C32T", "C64T", "Zq0", "Zq1", "X", "XT",
                      "U", "Up", "X2", "XT2"):
                d[n] = t16("%s_%d" % (n, g))
            for n in ("Z", "g_", "E", "es", "ei", "ed"):
                d[n] = t32("%s_%d" % (n, g))
            inner.append(d)
        for g in range(NSLOT):
            d = dict(inner[g % GRP])
            for n in ("qT", "kT", "kt", "vt", "zs", "Qd", "Kd", "PT", "X3"):
                d[n] = t16("%s_%d" % (n, g))
            d["bg"] = sb(c, st, "bg_%d" % g, (128, 8), F32)
            d["sm"] = sb(c, st, "sm_%d" % g, (128, 32), F32)
            slots.append(d)
        rb = Rot([t16("rb%d" % i) for i in range(2)])
        vn = Rot([t16("vn%d" % i) for i in range(2)])
        osb = Rot([t32("osb%d" % i) for i in range(2)])
        osq = Rot([t16("osq%d" % i) for i in range(2)])
        ors = Rot([t32("ors%d" % i) for i in range(2)])
        oo = Rot([t16("oo%d" % i) for i in range(2)])
        pp = Rot([ps(c, st, "p2_%d" % i, (128, 4, 128), F32) for i in range(6)])
        ptb = Rot([ps(c, st, "p2_tb%d" % i, (128, 8, 128), BF16) for i in range(2)])

        def fill_tri(t, op):
            P.op("pool", lambda e: e.memset(t[:], 1.0), writes=[t])
            P.op("pool", lambda e: e.affine_select(out=t[:], in_=t[:], pattern=[[0, 4], [1, 128]], compare_op=op,
                                                   fill=0.0, base=0, channel_multiplier=-1), reads=[t], writes=[t])

        def fill_block(t, bs):
            P.op("pool", lambda e: e.memset(t[:], 1.0), writes=[t])
            for a in range(128 // bs):
                v = t[:, :, a * bs:(a + 1) * bs]
                P.op("pool", lambda e, v=v, a=a: e.affine_select(out=v, in_=v, pattern=[[0, 4], [0, bs]],
                                                                  compare_op=ALU.is_ge, fill=0.0, base=-a * bs,
                                                                  channel_multiplier=1), reads=[t], writes=[t])
                P.op("pool", lambda e, v=v, a=a: e.affine_select(out=v, in_=v, pattern=[[0, 4], [0, bs]],
                                                                  compare_op=ALU.is_ge, fill=0.0, base=a * bs + bs - 1,
                                                                  channel_multiplier=-1), reads=[t], writes=[t])

        fill_tri(Mst, ALU.is_gt)
        fill_tri(Min, ALU.is_ge)
        fill_block(M32, 32)
        fill_block(M64, 64)
        P.tt("pool", Mc32[:], M64[:], M32[:], ALU.subtract, reads=[M64, M32], writes=[Mc32])
        P.ts("pool", Mc64[:], M64[:], -1.0, 1.0, ALU.mult, ALU.add, reads=[M64], writes=[Mc64])
        P.op("pool", lambda e: e.memset(ones32[:], 1.0), writes=[ones32])
        P.op("pool", lambda e: e.memset(S[:], 0.0), writes=[S])
        P.op("pool", lambda e: e.memset(Sb[:], 0.0), writes=[Sb])
        P.dma(normg[:], c.norm_g.rearrange("(p o) -> p o", o=1), writes=[normg])

        def pre(ci, delay=0):
            for _ in range(delay):
                yield
            c0, C = chunk_range(ci)
            d = slots[ci % NSLOT]
            qT, kT, kt, vt, zs, bg, s_ = d["qT"], d["kT"], d["kt"], d["vt"], d["zs"], d["bg"], d["sm"]
            P.dma(qT[:, :, :C], c.qT_dn[:, :, c0:c0 + C].rearrange("h p t -> p h t"), writes=[qT])
            P.dma(kT[:, :, :C], c.kT_dn[:, :, c0:c0 + C].rearrange("h p t -> p h t"), writes=[kT])
            P.dma(kt[:C, :, :], c.k_dn[c0:c0 + C, :].rearrange("t (h d) -> t h d", h=4), writes=[kt])
            P.dma(vt[:C, :, :], c.v_dn[c0:c0 + C, :].rearrange("t (h d) -> t h d", h=4), writes=[vt])
            P.dma(zs[:, :, :C], c.zsT[:, :, c0:c0 + C].rearrange("h p t -> p h t"), writes=[zs])
            P.dma(bg[:C, :], c.bgs[c0:c0 + C, :], writes=[bg])
            yield
            g_, E, es, ei, ed = d["g_"], d["E"], d["es"], d["ei"], d["ed"]
            for h in range(4):
                P.act(g_[:C, h, :], ones32[:C, :], AF.Copy, reads=[ones32, bg], writes=[g_], scale=bg[:C, 4 + h:5 + h])
            pd = pp.next()
            for h in range(4):
                P.mm(pd[:, h, :C], g_[:C, h, :], Min[:C, 0, :C], reads=[g_, Min], writes=[pd])
            pc_ = pp.next()
            P.mm(pc_[:C, 0, 0:4], Min[:C, 0, :C], bg[:C, 4:8], reads=[Min, bg], writes=[pc_])
            P.copy("dve", s_[:C, 0:4], pc_[:C, 0, 0:4], reads=[pc_], writes=[s_])
            P.ts("dve", s_[:C, 4:8], pc_[:C, 0, 0:4], -1.0, None, ALU.mult, reads=[pc_], writes=[s_])
            P.act(s_[:C, 8:12], s_[:C, 0:4], AF.Exp, reads=[s_], writes=[s_])
            P.ts("dve", s_[:C, 8:12], s_[:C, 8:12], -1.0, None, ALU.mult, reads=[s_], writes=[s_])
            P.tt("dve", s_[:C, 12:16], pd[:C, :, C - 1], s_[:C, 0:4], ALU.subtract, reads=[pd, s_], writes=[s_])
            P.act(s_[:C, 12:16], s_[:C, 12:16], AF.Exp, reads=[s_], writes=[s_])
            P.act(s_[:, 16:20], pd[:, :, C - 1], AF.Exp, reads=[pd], writes=[s_])
            for h in range(4):
                P.ts("dve", E[:C, h, :C], pd[:C, h, :C], s_[:C, 4 + h:5 + h], 0.0, ALU.add, ALU.min,
                     reads=[pd, s_], writes=[E])
            P.act(ed[:, :, :C], pd[:, :, :C], AF.Exp, reads=[pd], writes=[ed])
            yield
            P.act(E[:C, :, :C], E[:C, :, :C], AF.Exp, reads=[E], writes=[E])
            P.tt("pool", es[:C, :, :C], E[:C, :, :C], Mst[:C, :, :C], ALU.mult, reads=[E, Mst], writes=[es])
            P.tt("pool", ei[:C, :, :C], E[:C, :, :C], Min[:C, :, :C], ALU.mult, reads=[E, Min], writes=[ei])
            Qd, Kd, PT = d["Qd"], d["Kd"], d["PT"]
            P.tt("pool", Qd[:, :, :C], qT[:, :, :C], ed[:, :, :C], ALU.mult, reads=[qT, ed], writes=[Qd])
            for h in range(4):
                P.act(Kd[:C, h, :], kt[:C, h, :], AF.Copy, reads=[kt, s_], writes=[Kd], scale=s_[:C, 12 + h:13 + h])
            yield
            pG = pp.next()
            for h in range(4):
                P.mm(pG[:C, h, :C], kT[:, h, :C], kT[:, h, :C], reads=[kT], writes=[pG])
            pQK = pp.next()
            for h in range(4):
                P.mm(pQK[:C, h, :C], kT[:, h, :C], qT[:, h, :C], reads=[kT, qT], writes=[pQK])
            P.tt("dve", PT[:C, :, :C], pQK[:C, :, :C], ei[:C, :, :C], ALU.mult, reads=[pQK, ei], writes=[PT])
            N, NT = d["N"], d["NT"]
            for h in range(4):
                P.stt(N[:C, h, :C], pG[:C, h, :C], bg[:C, h:h + 1], es[:C, h, :C], ALU.mult, ALU.mult,
                      reads=[pG, bg, es], writes=[N])
            yield
            pt_ = ptb.next()
            for h in range(4):
                P.tr(pt_[:C, h, :C], N[:C, h, :C], c.identb[:C, :C], reads=[N, c.identb], writes=[pt_])
            P.copy("act", NT[:C, :, :C], pt_[:C, 0:4, :C], reads=[pt_], writes=[NT])
            Nd, NdT = d["Nd0"], d["NdT0"]
            P.tt("pool", Nd[:C, :, :C], N[:C, :, :C], M32[:C, :, :C], ALU.mult, reads=[N, M32], writes=[Nd])
            P.tt("pool", d["C32"][:C, :, :C], N[:C, :, :C], Mc32[:C, :, :C], ALU.mult, reads=[N, Mc32],
                 writes=[d["C32"]])
            Z, Zq = d["Z"], d["Zq0"]
            P.tt("dve", Z[:C, :, :C], c.ident4[:C, :, :C], Nd[:C, :, :C], ALU.subtract, reads=[c.ident4, Nd],
                 writes=[Z])
            yield
            P.tt("dve", NdT[:C, :, :C], NT[:C, :, :C], M32[:C, :, :C], ALU.mult, reads=[NT, M32], writes=[NdT])
            P.tt("dve", d["C32T"][:C, :, :C], NT[:C, :, :C], Mc32[:C, :, :C], ALU.mult, reads=[NT, Mc32],
                 writes=[d["C32T"]])
            P.tt("pool", d["C64T"][:C, :, :C], NT[:C, :, :C], Mc64[:C, :, :C], ALU.mult, reads=[NT, Mc64],
                 writes=[d["C64T"]])
            P.copy("act", Zq[:C, :, :C], Z[:C, :, :C], reads=[Z], writes=[Zq])
            yield
            for lvl in range(1, 5):
                pNT = pp.next()
                for h in range(4):
                    P.mm(pNT[:C, h, :C], Nd[:C, h, :C], NdT[:C, h, :C], reads=[Nd, NdT], writes=[pNT])
                if lvl < 4:
                    pN = pp.next()
                    for h in range(4):
                        P.mm(pN[:C, h, :C], NdT[:C, h, :C], Nd[:C, h, :C], reads=[Nd, NdT], writes=[pN])
                NdT = d["NdT%d" % (lvl % 2)]
                P.copy("act", NdT[:C, :, :C], pNT[:C, :, :C], reads=[pNT], writes=[NdT])
                if lvl < 4:
                    Nd = d["Nd%d" % (lvl % 2)]
                    P.copy("dve", Nd[:C, :, :C], pN[:C, :, :C], reads=[pN], writes=[Nd])
                yield
                pZ = pp.next()
                for h in range(4):
                    P.mm(pZ[:C, h, :C], NdT[:C, h, :C], Zq[:C, h, :C], reads=[NdT, Zq], writes=[pZ])
                P.tt("dve", Z[:C, :, :C], Z[:C, :, :C], pZ[:C, :, :C], ALU.add, reads=[Z, pZ], writes=[Z])
                Zq = d["Zq%d" % (lvl % 2)] if lvl < 4 else d["X"]
                P.copy("act", Zq[:C, :, :C], Z[:C, :, :C], reads=[Z], writes=[Zq])
                yield
            X, XT = d["X"], d["XT"]
            pt_ = ptb.next()
            for h in range(4):
                P.tr(pt_[:C, h, :C], X[:C, h, :C], c.identb[:C, :C], reads=[X, c.identb], writes=[pt_])
            pU = pp.next()
            for h in range(4):
                P.mm(pU[:C, h, :C], d["C32T"][:C, h, :C], X[:C, h, :C], reads=[d["C32T"], X], writes=[pU])
            P.copy("act", XT[:C, :, :C], pt_[:C, 0:4, :C], reads=[pt_], writes=[XT])
            P.copy("dve", d["U"][:C, :, :C], pU[:C, :, :C], reads=[pU], writes=[d["U"]])
            yield
            pUp = pp.next()
            for h in range(4):
                P.mm(pUp[:C, h, :C], d["C32"][:C, h, :C], XT[:C, h, :C], reads=[d["C32"], XT], writes=[pUp])
            pW = pp.next()
            for h in range(4):
                P.mm(pW[:C, h, :C], XT[:C, h, :C], d["U"][:C, h, :C], reads=[XT, d["U"]], writes=[pW])
            P.copy("act", d["Up"][:C, :, :C], pUp[:C, :, :C], reads=[pUp], writes=[d["Up"]])
            P.tt("dve", d["X2"][:C, :, :C], X[:C, :, :C], pW[:C, :, :C], ALU.subtract, reads=[X, pW],
                 writes=[d["X2"]])
            yield
            pWp = pp.next()
            for h in range(4):
                P.mm(pWp[:C, h, :C], X[:C, h, :C], d["Up"][:C, h, :C], reads=[X, d["Up"]], writes=[pWp])
            pU2 = pp.next()
            for h in range(4):
                P.mm(pU2[:C, h, :C], d["C64T"][:C, h, :C], d["X2"][:C, h, :C], reads=[d["C64T"], d["X2"]],
                     writes=[pU2])
            P.tt("dve", d["XT2"][:C, :, :C], XT[:C, :, :C], pWp[:C, :, :C], ALU.subtract, reads=[XT, pWp],
                 writes=[d["XT2"]])
            P.copy("act", d["U"][:C, :, :C], pU2[:C, :, :C], reads=[pU2], writes=[d["U"]])
            yield
            pW2 = pp.next()
            for h in range(4):
                P.mm(pW2[:C, h, :C], d["XT2"][:C, h, :C], d["U"][:C, h, :C], reads=[d["XT2"], d["U"]], writes=[pW2])
            P.tt("dve", d["X3"][:C, :, :C], d["X2"][:C, :, :C], pW2[:C, :, :C], ALU.subtract, reads=[d["X2"], pW2],
                 writes=[d["X3"]])

        def scan(ci):
            c0, C = chunk_range(ci)
            d = slots[ci % NSLOT]
            kT, vt, zs, bg, s_, Qd, Kd, PT, Zq = (d["kT"], d["vt"], d["zs"], d["bg"], d["sm"], d["Qd"], d["Kd"],
                                                  d["PT"], d["X3"])
            pKS = pp.next()
            for h in range(4):
                P.mm(pKS[:C, h, :], kT[:, h, :C], Sb[:, h, :], reads=[kT, Sb], writes=[pKS])
            r_ = rb.next()
            for h in range(4):
                P.stt(r_[:C, h, :], pKS[:C, h, :], s_[:C, 8 + h:9 + h], vt[:C, h, :], ALU.mult, ALU.add,
                      reads=[pKS, s_, vt], writes=[r_])
            yield
            pVN = pp.next()
            for h in range(4):
                P.mm(pVN[:C, h, :], Zq[:C, h, :C], r_[:C, h, :], reads=[Zq, r_], writes=[pVN])
            v_ = vn.next()
            for h in range(4):
                if h < 2:
                    P.ts("dve", v_[:C, h, :], pVN[:C, h, :], bg[:C, h:h + 1], None, ALU.mult,
                         reads=[pVN, bg], writes=[v_])
                else:
                    P.act(v_[:C, h, :], pVN[:C, h, :], AF.Copy, reads=[pVN, bg], writes=[v_], scale=bg[:C, h:h + 1])
            yield
            po = pp.next()
            for h in range(4):
                P.mm(po[:, h, :C], Sb[:, h, :], Qd[:, h, :C], start=True, stop=False, reads=[Sb, Qd], writes=[po])
                P.mm(po[:, h, :C], v_[:C, h, :], PT[:C, h, :C], start=False, stop=True, reads=[v_, PT], writes=[po])
            pSN = pp.next()
            for h in range(4):
                P.mm(pSN[:, h, :], Kd[:C, h, :], v_[:C, h, :], reads=[Kd, v_], writes=[pSN])
            for h in range(4):
                P.stt(S[:, h, :], S[:, h, :], s_[:, 16 + h:17 + h], pSN[:, h, :], ALU.mult, ALU.add,
                      reads=[S, s_, pSN], writes=[S])
            P.copy("act", Sb[:], S[:], reads=[S], writes=[Sb])
            o_, q_, rs_, oo_ = osb.next(), osq.next(), ors.next(), oo.next()
            P.copy("act", o_[:, :, :C], po[:, :, :C], reads=[po], writes=[o_])
            P.act(q_[:, :, :C], po[:, :, :C], AF.Square, reads=[po], writes=[q_])
            yield
            pss = pp.next()
            for h in range(4):
                P.mm(pss[:, h, :C], c.onesb[:, :], q_[:, h, :C], reads=[c.onesb, q_], writes=[pss])
            P.act(rs_[:, :, :C], pss[:, :, :C], AF.Ln, reads=[pss], writes=[rs_], scale=1.0 / 128.0, bias=RMS_EPS)
            P.act(rs_[:, :, :C], rs_[:, :, :C], AF.Exp, reads=[rs_], writes=[rs_], scale=-0.5)
            P.tt("dve", o_[:, :, :C], o_[:, :, :C], rs_[:, :, :C], ALU.mult, reads=[o_, rs_], writes=[o_])
            P.stt(oo_[:, :, :C], o_[:, :, :C], normg[:, 0:1], zs[:, :, :C], ALU.mult, ALU.mult,
                  reads=[o_, normg, zs], writes=[oo_])
            P.dma(c.odnT[:, :, c0:c0 + C].rearrange("h p t -> p h t"), oo_[:, :, :C], reads=[oo_], writes=[Region()])
            yield

        def scan_seq(group):
            for ci in group:
                yield from scan(ci)

        def run_lockstep(gens):
            while gens:
                for g in list(gens):
                    try:
                        next(g)
                    except StopIteration:
                        gens.remove(g)

        groups = [list(range(g0, min(N_CH, g0 + GRP))) for g0 in range(0, N_CH, GRP)]
        def pre_item(ci):
            after = []
            if ci - GRP >= 0:
                after.append("pre%d" % (ci - GRP))
            if ci - NSLOT >= 0:
                after.append("scan%d" % (ci - NSLOT))
            return (None, (lambda _s, ci=ci: pre(ci)), "pre%d" % ci, tuple(after))

        def scan_item(ci):
            after = ["pre%d" % ci] + (["scan%d" % (ci - 1)] if ci > 0 else [])
            return (None, (lambda _s, ci=ci: scan(ci)), "scan%d" % ci, tuple(after))

        queue = [pre_item(ci) for ci in range(min(GRP, N_CH))]
        for ci in range(N_CH):
            queue.append(scan_item(ci))
            if ci + GRP < N_CH:
                queue.append(pre_item(ci + GRP))
        P.run_sched(queue)
        P.barrier()


def run_lockstep(gens):
    gens = list(gens)
    while gens:
        for g in list(gens):
            try:
                next(g)
            except StopIteration:
                gens.remove(g)


USE_SCHED = True


def phase3(c):
    P, nc = c.P, c.nc
    with ExitStack() as st:
        KT = sb(c, st, "KT", (128, 4, L), BF16)
        VA = sb(c, st, "VA", (128, 33, 512), BF16)
        VB = sb(c, st, "VB", (128, 33, 512), BF16)
        negTri = sb(c, st, "negTri", (128, 128), BF16)
        negOnes = sb(c, st, "negOnes", (128, 128), BF16)
        zer = sb(c, st, "zer", (128, 128), BF16)
        tmpm = sb(c, st, "tmpm", (128, 128), F32)
        QT = [sb(c, st, "QT%d" % i, (128, 4, 512), BF16) for i in range(2)]
        hs = []
        for i in range(8):
            hs.append({"e": sb(c, st, "e_%d" % i, (128, 512), F32), "sp": sb(c, st, "sp_%d" % i, (128, 512), BF16),
                       "sp2": sb(c, st, "sp2_%d" % i, (128, 512), BF16),
                       "W": sb(c, st, "W_%d" % i, (128, 512), BF16), "SP": sb(c, st, "SP_%d" % i, (128, 512), BF16),
                       "x": sb(c, st, "x_%d" % i, (128, 512), F32)})
        pos = [ps(c, st, "p3o_%d" % i, (128, 512), F32) for i in range(4)]
        ot = Rot([sb(c, st, "ot%d" % i, (128, 512), BF16) for i in range(3)])
        pz = Rot([ps(c, st, "p3z_%d" % i, (128, 512), F32) for i in range(2)])
        pa = Rot([ps(c, st, "p3a_%d" % i, (128, 512), F32) for i in range(2)])

        P.op("pool", lambda e: e.memset(tmpm[:], -1.0), writes=[tmpm])
        P.op("pool", lambda e: e.affine_select(out=tmpm[:], in_=tmpm[:], pattern=[[-1, 128]], compare_op=ALU.is_ge,
                                               fill=0.0, base=0, channel_multiplier=1), reads=[tmpm], writes=[tmpm])
        P.copy("pool", negTri[:], tmpm[:], reads=[tmpm], writes=[negTri])
        P.op("pool", lambda e: e.memset(negOnes[:], -1.0), writes=[negOnes])
        P.op("pool", lambda e: e.memset(zer[:], 0.0), writes=[zer])
        P.op("pool", lambda e: e.memset(tmpm[:], 1.0), reads=[negTri], writes=[tmpm])
        P.op("pool", lambda e: e.affine_select(out=tmpm[:], in_=tmpm[:], pattern=[[1, 128]], compare_op=ALU.is_gt,
                                               fill=0.0, base=0, channel_multiplier=-1), reads=[tmpm], writes=[tmpm])
        KT_r = [Region("KT%d" % i) for i in range(4)]
        for cc in range(4):
            P.dma(KT[:, cc, :], c.kT_sb[cc, :, :], writes=[KT_r[cc]])
        V_r = [Region("V%d" % i) for i in range(9)]
        Vall = Region("Vall")
        P.op("pool", lambda e: e.memset(VA[:], 0.0), writes=[Vall])
        P.dma(VB[:NMETA, 0, :], c.v_sb[0:NMETA, :], writes=[V_r[0]])
        for g in range(8):
            P.dma(VB[:, 1 + 4 * g:5 + 4 * g, :],
                  c.v_sb[NMETA + 512 * g:NMETA + 512 * (g + 1), :].rearrange("(b p) f -> p b f", p=128),
                  writes=[V_r[1 + g]])
        VAv = VA[:].rearrange("p b (c two d) -> p b c two d", two=2, d=64)
        VBv = VB[:].rearrange("p b (c two d) -> p b c two d", two=2, d=64)
        P.copy("dve", VAv[:NMETA, 0:1, :, 0, :], VBv[:NMETA, 0:1, :, 0, :], reads=V_r + [Vall], writes=[Vall])
        P.op("dve", lambda e: e.memset(VBv[:NMETA, 0:1, :, 0, :], 0.0), reads=[Vall], writes=[Vall])
        for g in range(8):
            eng = "dve" if g % 2 == 0 else "pool"
            blk = slice(1 + 4 * g, 5 + 4 * g)
            P.copy(eng, VAv[:, blk, :, 0, :], VBv[:, blk, :, 0, :], reads=V_r + [Vall], writes=[Vall])
            P.op(eng, lambda e, blk=blk: e.memset(VBv[:, blk, :, 0, :], 0.0), reads=[Vall], writes=[Vall])
        vreg = lambda kb: Vall
        done = {}

        def evac(j, h, po):
            t0, Tn = st_range(j)
            cc = h // 2
            done[(j, cc)] = done.get((j, cc), 0) + 1
            if done[(j, cc)] == 2:
                o = ot.next()
                P.copy("dve", o[:, :Tn], po[:, :Tn], reads=[po], writes=[o])
                P.dma(c.osbT[cc, :, t0:t0 + Tn], o[:, :Tn], reads=[o], writes=[Region()])

        def head_stream(h, delay):
            for _ in range(delay):
                yield
            cc, pb = h // 2, 64 * (h % 2)
            slot = hs[h]
            e_, W_, SP_, x_ = slot["e"], slot["W"], slot["SP"], slot["x"]
            sps = (slot["sp"], slot["sp2"])
            po = pos[cc]
            Vh = VA if h % 2 == 0 else VB
            for j in range(N_ST):
                t0, Tn = st_range(j)
                Q = QT[j % 2]
                if h == 0:
                    P.dma(Q[:, :, :Tn], c.qT_sb[:, :, t0:t0 + Tn].rearrange("c p t -> p c t"), writes=[Q])
                kbs = [0] if j == 0 else list(range(4 * j, -1, -1))
                P.op("dve", lambda e: e.memset(SP_[:], 0.0), writes=[SP_])
                if h % 2 == 1:
                    P.mm(po[:, :Tn], zer[:, :], Q[:, cc, :Tn], start=True, stop=False, reads=[zer, Q], writes=[po])
                for idx, kb in enumerate(kbs):
                    k0, KC = chunk_range(kb)
                    diag = (j == 0) or (kb >= 4 * j - 3)
                    q0 = 0 if j == 0 else (128 * (kb - (4 * j - 3)) if diag else 0)
                    N = Tn - q0
                    dw = min(128, N)
                    first, last = idx == 0, idx == len(kbs) - 1
                    sp_ = sps[idx % 2]
                    kt_ap = KT[pb:pb + 64, cc, k0:k0 + KC]
                    q_ap = Q[pb:pb + 64, cc, q0:q0 + N]
                    z = pz.next()
                    P.mm(z[:KC, :N], kt_ap, q_ap, reads=[KT_r[cc], Q], writes=[z])
                    P.act(e_[:KC, :N], z[:KC, :N], AF.Exp, reads=[z], writes=[e_])
                    if not first:
                        pKC, pq0, pN, psp = prev
                        P.tt("dve", SP_[:pKC, pq0:pq0 + pN], SP_[:pKC, pq0:pq0 + pN], psp[:pKC, :pN], ALU.add,
                             reads=[SP_, psp], writes=[SP_])
                    if diag:
                        P.tt("dve", e_[:KC, 0:dw], e_[:KC, 0:dw], tmpm[:KC, 0:dw], ALU.mult, reads=[e_, tmpm],
                             writes=[e_])
                    yield
                    P.act(sp_[:KC, :N], e_[:KC, :N], AF.Ln, reads=[e_], writes=[sp_], bias=1.0)
                    yield
                    a = pa.next()
                    P.mm(a[:KC, :N], negTri[:KC, :KC], sp_[:KC, :N], start=True, stop=first, reads=[negTri, sp_],
                         writes=[a])
                    if not first:
                        P.mm(a[:KC, :N], negOnes[:, :KC], SP_[:, q0:q0 + N], start=False, stop=True,
                             reads=[negOnes, SP_], writes=[a])
                    P.act(x_[:KC, :N], a[:KC, :N], AF.Exp, reads=[a], writes=[x_])
                    yield
                    P.tt("dve", W_[:KC, :N], x_[:KC, :N], e_[:KC, :N], ALU.mult, reads=[x_, e_], writes=[W_])
                    P.mm(po[:, q0:q0 + N], Vh[:KC, kb, cc * 128:(cc + 1) * 128], W_[:KC, :N], start=False,
                         stop=(last and h % 2 == 1),
                         reads=[vreg(kb), W_], writes=[po])
                    prev = (KC, q0, N, sp_)
                    yield
                evac(j, h, po)

        run_lockstep([head_stream(h, 0 if h < 4 else 2) for h in range(8)])
        P.barrier()


def layer_norm_tile(P, xt, rows, stt_, gbc, bbc, out):
    P.op("dve", lambda e: e.bn_stats(out=stt_[:rows, 0:6], in_=xt[:rows, 0:512]), reads=[xt], writes=[stt_])
    P.op("dve", lambda e: e.bn_stats(out=stt_[:rows, 6:12], in_=xt[:rows, 512:1024]), reads=[xt], writes=[stt_])
    P.op("dve", lambda e: e.bn_aggr(out=stt_[:rows, 12:14], in_=stt_[:rows, 0:12]), reads=[stt_], writes=[stt_])
    rsqrt(P, stt_[:rows, 14:15], stt_[:rows, 13:14], LN_EPS, [stt_], stt_)
    P.ts("dve", xt[:rows, :], xt[:rows, :], stt_[:rows, 12:13], stt_[:rows, 14:15], ALU.subtract, ALU.mult,
         reads=[xt, stt_], writes=[xt])
    P.tt("pool", xt[:rows, :], xt[:rows, :], gbc[:rows, :], ALU.mult, reads=[xt, gbc], writes=[xt])
    P.tt("pool", out[:rows, :], xt[:rows, :], bbc[:rows, :], ALU.add, reads=[xt, bbc], writes=[out])


def run_window2(queue, slotsets):
    free = {k: list(range(len(v))) for k, v in slotsets.items()}
    active = []
    queue = list(queue)
    while queue or active:
        while queue and queue[0] != "drain" and free[queue[0][0]]:
            cls, mk = queue.pop(0)
            si = free[cls].pop(0)
            active.append((mk(slotsets[cls][si]), cls, si))
        if queue and queue[0] == "drain" and not active:
            queue.pop(0)
            continue
        for item in list(active):
            try:
                next(item[0])
            except StopIteration:
                active.remove(item)
                free[item[1]].append(item[2])


def phase4(c):
    P, nc = c.P, c.nc
    with ExitStack() as st:
        Wbd = sb(c, st, "Wbd", (128, 4, D), BF16)
        Wbs = sb(c, st, "Wbs", (128, 4, D), BF16)
        Wout = sb(c, st, "Wout", (128, 8, D), BF16)
        gbc = sb(c, st, "g1bc", (128, D), F32)
        bbc = sb(c, st, "b1bc", (128, D), F32)
        Wr = sb(c, st, "Wr", (128, 8, 36), F32)
        rbb = sb(c, st, "rbb", (128, 36), F32)
        odn = [sb(c, st, "odn%d" % i, (128, 4, 512), BF16) for i in range(2)]
        osb_ = [sb(c, st, "osb4_%d" % i, (128, 4, 512), BF16) for i in range(2)]
        mT = [sb(c, st, "mT%d" % i, (128, 8, 512), BF16) for i in range(2)]
        mreg = [[Region("mT%d_%d" % (i, d_)) for d_ in range(8)] for i in range(2)]
        dcs = [{"ta": sb(c, st, "tA%d" % i, (128, 512), F32), "tb": sb(c, st, "tB%d" % i, (128, 512), F32),
                "g": sb(c, st, "gdc%d" % i, (128, 2, 512), BF16)} for i in range(5)]
        subs = [{"h0": sb(c, st, "h0b%d" % i, (128, D), F32), "h1": sb(c, st, "h1b%d" % i, (128, D), F32),
                 "st": sb(c, st, "st4_%d" % i, (128, 16), F32), "hf": sb(c, st, "hTf%d" % i, (128, 8, 128), F32),
                 "hb": sb(c, st, "hTb%d" % i, (128, 8, 128), BF16), "r": sb(c, st, "rt%d" % i, (128, 128), F32)}
                for i in range(4)]
        pAB = Rot([ps(c, st, "p4ab%d" % i, (128, 512), F32) for i in range(3)])
        pM = ps(c, st, "p4m", (128, D), F32)
        pT = ps(c, st, "p4T", (128, D), F32)
        pR = ps(c, st, "p4R", (128, 512), F32)

        for k in range(4):
            P.dma(Wbd[:, k, :], c.w_bdn[k * 128:(k + 1) * 128, :], writes=[Region()], eng="pool")
            P.dma(Wbs[:, k, :], c.w_bsb[k * 128:(k + 1) * 128, :], writes=[Region()], eng="pool")
        for k in range(8):
            P.dma(Wout[:, k, :], c.w_out[k * 128:(k + 1) * 128, :], writes=[Region()], eng="pool")
        P.dma(gbc[:], c.ln1_g.partition_broadcast(128), writes=[gbc])
        P.dma(bbc[:], c.ln1_b.partition_broadcast(128), writes=[bbc])
        P.dma(Wr[:, :, 0:4], c.rg_w.rearrange("(k p) g -> p k g", p=128), writes=[Region()])
        for g in range(NG):
            P.dma(Wr[:, :, 4 + 8 * g:12 + 8 * g], c.re_w[g].rearrange("(k p) e -> p k e", p=128), writes=[Region()])
        P.dma(rbb[:, 0:4], c.rg_b.partition_broadcast(128), writes=[Region()])
        P.dma(rbb[:, 4:36], c.re_b.rearrange("g e -> (g e)").partition_broadcast(128), writes=[Region()])
        P.barrier()

        def load_st(j):
            t0, Tn = st_range(j)
            a_, b_ = odn[j % 2], osb_[j % 2]
            P.dma(a_[:], c.odnT[:, :, t0:t0 + Tn].rearrange("c p t -> p c t"), writes=[a_])
            P.dma(b_[:], c.osbT[:, :, t0:t0 + Tn].rearrange("c p t -> p c t"), writes=[b_])

        def gen_dc(j, dc, slot):
            a_, b_, m_ = odn[j % 2], osb_[j % 2], mT[j % 2]
            t0, Tn = st_range(j)
            if dc == 0:
                load_st(j)
            ta, tb, g_ = slot["ta"], slot["tb"], slot["g"]
            P.dma(g_[:, 0, :], c.gsT[dc, :, t0:t0 + Tn], writes=[g_])
            P.dma(g_[:, 1, :], c.gsT[8 + dc, :, t0:t0 + Tn], writes=[g_])
            pa, pb = pAB.next(), pAB.next()
            for k in range(4):
                P.mm(pa[:], Wbd[:, k, dc * 128:(dc + 1) * 128], a_[:, k, :], start=(k == 0), stop=(k == 3),
                     reads=[a_], writes=[pa])
            for k in range(4):
                P.mm(pb[:], Wbs[:, k, dc * 128:(dc + 1) * 128], b_[:, k, :], start=(k == 0), stop=(k == 3),
                     reads=[b_], writes=[pb])
            P.tt("dve", ta[:], pa[:], g_[:, 0, :], ALU.mult, reads=[pa, g_], writes=[ta])
            P.tt("dve", tb[:], pb[:], g_[:, 1, :], ALU.mult, reads=[pb, g_], writes=[tb])
            yield
            P.tt("pool" if dc % 2 else "dve", m_[:, dc, :], ta[:], tb[:], ALU.add, reads=[ta, tb],
                 writes=[mreg[j % 2][dc]])
            yield

        def gen_sub(j, s_i, slot):
            t0, Tn = st_range(j)
            m_ = mT[j % 2]
            r0 = t0 + s_i * 128
            x0 = r0 - NMETA
            h0t, h1t, stt_, hf, hb_, r = slot["h0"], slot["h1"], slot["st"], slot["hf"], slot["hb"], slot["r"]
            P.dma(h0t[:], c.h0s[r0:r0 + 128, :], writes=[h0t])
            for half in range(2):
                for k in range(8):
                    P.mm(pM[:, half * 512:(half + 1) * 512], m_[:, k, s_i * 128:(s_i + 1) * 128],
                         Wout[:, k, half * 512:(half + 1) * 512], start=(k == 0), stop=(k == 7),
                         reads=[mreg[j % 2][k]], writes=[pM])
            P.stt(h0t[:], h0t[:], ALPHA, pM[:], ALU.mult, ALU.add, reads=[h0t, pM], writes=[h0t])
            yield
            P.op("dve", lambda e: e.bn_stats(out=stt_[:, 0:6], in_=h0t[:, 0:512]), reads=[h0t], writes=[stt_])
            P.op("dve", lambda e: e.bn_stats(out=stt_[:, 6:12], in_=h0t[:, 512:1024]), reads=[h0t], writes=[stt_])
            P.op("dve", lambda e: e.bn_aggr(out=stt_[:, 12:14], in_=stt_[:, 0:12]), reads=[stt_], writes=[stt_])
            P.act(stt_[:, 14:15], stt_[:, 13:14], AF.Ln, reads=[stt_], writes=[stt_], bias=LN_EPS)
            yield
            P.act(stt_[:, 14:15], stt_[:, 14:15], AF.Exp, reads=[stt_], writes=[stt_], scale=-0.5)
            P.stt(stt_[:, 15:16], stt_[:, 12:13], -1.0, stt_[:, 14:15], ALU.mult, ALU.mult, reads=[stt_],
                  writes=[stt_])
            yield
            P.act(h0t[:], h0t[:], AF.Identity, reads=[h0t, stt_], writes=[h0t], scale=stt_[:, 14:15],
                  bias=stt_[:, 15:16])
            yield
            P.tt("dve", h0t[:], h0t[:], gbc[:], ALU.mult, reads=[h0t, gbc], writes=[h0t])
            P.tt("pool", h1t[:], h0t[:], bbc[:], ALU.add, reads=[h0t, bbc], writes=[h1t])
            P.dma(c.h1s[x0:x0 + 128, :], h1t[:], reads=[h1t], writes=[Region()], eng="pool")
            yield
            for k in range(8):
                P.tr(pT[:, k * 128:(k + 1) * 128], h1t[:, k * 128:(k + 1) * 128], c.ident[:, :],
                     reads=[h1t, c.ident], writes=[pT])
            P.copy("act", hf[:], pT[:].rearrange("p (k t) -> p k t", k=8), reads=[pT], writes=[hf])
            yield
            P.copy("act", hb_[:], hf[:], reads=[hf], writes=[hb_])
            P.dma(c.h1T[:, :, x0:x0 + 128].rearrange("k p t -> p k t"), hb_[:], reads=[hb_], writes=[Region()], eng="pool")
            for k in range(8):
                P.mm(pR[:, 0:36], hf[:, k, :], Wr[:, k, :], start=(k == 0), stop=(k == 7), reads=[hf],
                     writes=[pR])
            P.tt("dve", r[:, 0:36], pR[:, 0:36], rbb[:, :], ALU.add, reads=[pR, rbb], writes=[r])
            yield
            yield from route(P, r)
            P.dma(c.combs[x0:x0 + 128, :], r[:, 96:128], reads=[r], writes=[Region()], eng="pool")

        def dc_item(j, dc):
            after = tuple("sub%d_%d" % (j - 2, s_i) for s_i in range(4)) if j - 2 >= 1 else ()
            return ("dc", (lambda sl_, j=j, dc=dc: gen_dc(j, dc, sl_)), "dc%d_%d" % (j, dc), after)

        def sub_item(j, s_i):
            return ("sub", (lambda sl_, j=j, s_i=s_i: gen_sub(j, s_i, sl_)), "sub%d_%d" % (j, s_i),
                    tuple("dc%d_%d" % (j, dc) for dc in range(8)))

        queue = [dc_item(1, dc) for dc in range(8)]
        for j in range(1, N_ST):
            a_items = [sub_item(j, s_i) for s_i in range(4)]
            b_items = [dc_item(j + 1, dc) for dc in range(8)] if j + 1 < N_ST else []
            while a_items or b_items:
                if a_items:
                    queue.append(a_items.pop(0))
                for _ in range(2):
                    if b_items:
                        queue.append(b_items.pop(0))
        P.run_sched(queue, {"dc": dcs, "sub": subs})
        P.barrier()


def route(P, r):
    R_, W_ = [r], [r]
    P.op("dve", lambda e: e.reduce_max(out=r[:, 36:37], in_=r[:, 0:4], axis=mybir.AxisListType.X), reads=R_, writes=W_)
    P.ts("dve", r[:, 37:38], r[:, 36:37], -1.0, None, ALU.mult, reads=R_, writes=W_)
    P.ts("dve", r[:, 43:47], r[:, 0:4], r[:, 36:37], None, ALU.is_equal, reads=R_, writes=W_)
    P.ts("dve", r[:, 48:56], r[:, 4:12], r[:, 43:44], None, ALU.mult, reads=R_, writes=W_)
    for g in range(1, NG):
        P.stt(r[:, 48:56], r[:, 4 + 8 * g:12 + 8 * g], r[:, 43 + g:44 + g], r[:, 48:56], ALU.mult, ALU.add,
              reads=R_, writes=W_)
    P.op("dve", lambda e: e.max(out=r[:, 56:64], in_=r[:, 48:56]), reads=R_, writes=W_)
    P.tt("dve", r[:, 64:65], r[:, 57:58], r[:, 56:57], ALU.subtract, reads=R_, writes=W_)
    yield
    P.act(r[:, 38:42], r[:, 0:4], AF.Exp, reads=R_, writes=W_, bias=r[:, 37:38])
    P.act(r[:, 64:65], r[:, 64:65], AF.Exp, reads=R_, writes=W_)
    yield
    P.op("dve", lambda e: e.reduce_sum(out=r[:, 42:43], in_=r[:, 38:42], axis=mybir.AxisListType.X), reads=R_,
         writes=W_)
    P.op("dve", lambda e: e.reciprocal(out=r[:, 42:43], in_=r[:, 42:43]), reads=R_, writes=W_)
    P.ts("dve", r[:, 65:66], r[:, 64:65], 1.0, None, ALU.add, reads=R_, writes=W_)
    P.op("dve", lambda e: e.reciprocal(out=r[:, 65:66], in_=r[:, 65:66]), reads=R_, writes=W_)
    P.tt("dve", r[:, 65:66], r[:, 65:66], r[:, 42:43], ALU.mult, reads=R_, writes=W_)
    P.tt("dve", r[:, 66:67], r[:, 65:66], r[:, 64:65], ALU.mult, reads=R_, writes=W_)
    P.ts("dve", r[:, 72:80], r[:, 48:56], r[:, 56:57], r[:, 65:66], ALU.is_equal, ALU.mult, reads=R_, writes=W_)
    P.ts("dve", r[:, 80:88], r[:, 48:56], r[:, 57:58], r[:, 66:67], ALU.is_equal, ALU.mult, reads=R_, writes=W_)
    P.tt("dve", r[:, 72:80], r[:, 72:80], r[:, 80:88], ALU.add, reads=R_, writes=W_)
    for g in range(NG):
        P.ts("dve", r[:, 96 + 8 * g:104 + 8 * g], r[:, 72:80], r[:, 43 + g:44 + g], None, ALU.mult, reads=R_,
             writes=W_)
    yield


def phase5(c):
    P, nc = c.P, c.nc
    NGRP = 4
    NEXP = NG * NE
    with ExitStack() as st:
        gbc = sb(c, st, "g2bc", (128, D), F32)
        bbc = sb(c, st, "b2bc", (128, D), F32)
        wgf = Rot([sb(c, st, "wgf%d" % i, (128, 8, FF), F32) for i in range(2)])
        wuf = Rot([sb(c, st, "wuf%d" % i, (128, 8, FF), F32) for i in range(2)])
        wdf = Rot([sb(c, st, "wdf%d" % i, (128, 2, D), F32) for i in range(2)])
        wgb = Rot([sb(c, st, "wgb%d" % i, (128, 8, FF), BF16) for i in range(2)])
        wub = Rot([sb(c, st, "wub%d" % i, (128, 8, FF), BF16) for i in range(2)])
        wdb = Rot([sb(c, st, "wdb%d" % i, (128, 2, D), BF16) for i in range(2)])
        hT = sb(c, st, "hT5", (128, 8, 1024), BF16)
        yaccs = [sb(c, st, "yacc%d" % i, (128, 8, D), F32) for i in range(2)]
        combs = [sb(c, st, "comb%d" % i, (128, 8, 32), F32) for i in range(2)]
        sg = Rot([sb(c, st, "sg%d" % i, (128, 512), F32) for i in range(2)])
        hid = Rot([sb(c, st, "hid%d" % i, (128, 2, 512), BF16) for i in range(2)])
        h1b = Rot([sb(c, st, "h1b5_%d" % i, (128, D), F32) for i in range(2)])
        ob = Rot([sb(c, st, "ob5_%d" % i, (128, D), F32) for i in range(2)])
        stats = Rot([sb(c, st, "st5_%d" % i, (128, 16), F32) for i in range(2)])
        pGU = Rot([ps(c, st, "p5gu%d" % i, (128, 512), F32) for i in range(4)])
        pY = Rot([ps(c, st, "p5y%d" % i, (128, D), F32) for i in range(2)])
        yreg = [[Region("yacc%d_%d" % (b_, i)) for i in range(8)] for b_ in range(2)]
        hreg = [Region("hT5_%d" % i) for i in range(2)]

        P.dma(gbc[:], c.ln2_g.partition_broadcast(128), writes=[gbc])
        P.dma(bbc[:], c.ln2_b.partition_broadcast(128), writes=[bbc])

        wts = {}

        def load_weights(grp, e_i):
            gf, uf, df = wgf.next(), wuf.next(), wdf.next()
            gb_, ub_, db_ = wgb.next(), wub.next(), wdb.next()
            P.dma(gf[:], c.e_gate[e_i].rearrange("(k p) f -> p k f", p=128), writes=[gf])
            P.dma(uf[:], c.e_up[e_i].rearrange("(k p) f -> p k f", p=128), writes=[uf])
            P.dma(df[:], c.e_down[e_i].rearrange("(k p) f -> p k f", p=128), writes=[df])
            P.copy("act", gb_[:], gf[:], reads=[gf], writes=[gb_])
            P.copy("pool", ub_[:], uf[:], reads=[uf], writes=[ub_])
            P.copy("act", db_[:, 0, :], df[:, 0, :], reads=[df], writes=[db_])
            P.copy("pool", db_[:, 1, :], df[:, 1, :], reads=[df], writes=[db_])
            wts[(grp, e_i)] = (gb_, ub_, db_)

        def gate_up(grp, e_i, half):
            gb_, ub_, db_ = wts[(grp, e_i)]
            hd = hid.next()
            for ffc in range(2):
                pg, pu = pGU.next(), pGU.next()
                for k in range(8):
                    P.mm(pg[:], gb_[:, k, ffc * 128:(ffc + 1) * 128], hT[:, k, half * 512:(half + 1) * 512],
                         start=(k == 0), stop=(k == 7), reads=[gb_, hreg[half]], writes=[pg])
                for k in range(8):
                    P.mm(pu[:], ub_[:, k, ffc * 128:(ffc + 1) * 128], hT[:, k, half * 512:(half + 1) * 512],
                         start=(k == 0), stop=(k == 7), reads=[ub_, hreg[half]], writes=[pu])
                s_ = sg.next()
                P.act(s_[:], pg[:], AF.Silu, reads=[pg], writes=[s_])
                P.tt("dve", hd[:, ffc, :], s_[:], pu[:], ALU.mult, reads=[s_, pu], writes=[hd])
            return hd

        def down(grp, e_i, half, hd):
            gb_, ub_, db_ = wts[(grp, e_i)]
            yacc, comb, yr = yaccs[grp % 2], combs[grp % 2], yreg[grp % 2]
            for s_i in range(4):
                ti = half * 4 + s_i
                py = pY.next()
                for hh in range(2):
                    for ffc in range(2):
                        P.mm(py[:, hh * 512:(hh + 1) * 512], hd[:, ffc, s_i * 128:(s_i + 1) * 128],
                             db_[:, ffc, hh * 512:(hh + 1) * 512], start=(ffc == 0), stop=(ffc == 1),
                             reads=[hd, db_], writes=[py])
                if e_i == 0:
                    P.ts("dve", yacc[:, ti, :], py[:], comb[:, ti, e_i:e_i + 1], None, ALU.mult,
                         reads=[py, comb], writes=[yr[ti]])
                else:
                    P.stt(yacc[:, ti, :], py[:], comb[:, ti, e_i:e_i + 1], yacc[:, ti, :], ALU.mult, ALU.add,
                          reads=[py, comb, yr[ti]], writes=[yr[ti]])

        def finish_tile(grp, ti):
            x0 = grp * 1024
            r0 = x0 + ti * 128
            yacc, yr = yaccs[grp % 2], yreg[grp % 2]
            h1t, o_, stt_ = h1b.next(), ob.next(), stats.next()
            P.dma(h1t[:], c.h1s[r0:r0 + 128, :], writes=[h1t], eng="pool")
            P.stt(h1t[:], h1t[:], ALPHA, yacc[:, ti, :], ALU.mult, ALU.add, reads=[h1t, yr[ti]], writes=[h1t])
            layer_norm_tile(P, h1t, 128, stt_, gbc, bbc, o_)
            P.dma(c.out[r0:r0 + 128, :], o_[:], reads=[o_], writes=[Region()], eng="pool")

        units = [(grp, e_i, half) for grp in range(NGRP) for e_i in range(NEXP) for half in range(2)]

        def prep_group(grp):
            x0 = grp * 1024
            for half in range(2):
                P.dma(hT[:, :, half * 512:(half + 1) * 512],
                      c.h1T[:, :, x0 + half * 512:x0 + (half + 1) * 512].rearrange("k p t -> p k t"),
                      writes=[hreg[half]])
            P.dma(combs[grp % 2][:], c.combs[x0:x0 + 1024, :].rearrange("(s p) e -> p s e", p=128),
                  writes=[combs[grp % 2]])

        prep_group(0)
        load_weights(0, 0)
        load_weights(0, 1)
        hd_prev = gate_up(*units[0])
        for ui, (grp, e_i, half) in enumerate(units):
            nxt = units[ui + 1] if ui + 1 < len(units) else None
            if nxt is not None:
                if nxt[2] == 0 and nxt[1] == 0:
                    prep_group(nxt[0])
                hd_next = gate_up(*nxt)
            down(grp, e_i, half, hd_prev)
            if nxt is not None:
                hd_prev = hd_next
                if nxt[2] == 0:
                    e2, g2 = nxt[1] + 1, nxt[0]
                    if e2 == NEXP:
                        e2, g2 = 0, g2 + 1
                    if g2 < NGRP:
                        load_weights(g2, e2)
            if grp > 0 and half == 1 and e_i < 8:
                finish_tile(grp - 1, e_i)
        for ti in range(8):
            finish_tile(NGRP - 1, ti)
        P.barrier()


_CACHE = {}


def _core_inputs(inputs, b):
    f32 = lambda a: np.ascontiguousarray(np.asarray(a, dtype=np.float32))
    m = {"x": f32(inputs["x"][b]), "meta_tokens": f32(inputs["meta_tokens"]),
         "ln_emb_g": f32(inputs["ln_emb_g"]), "ln_emb_b": f32(inputs["ln_emb_b"])}
    for n in ("w_in", "b_gate", "dn_conv_w", "dn_a_log", "dn_dt_bias", "dn_norm_g", "w_branch_dn", "w_branch_sb",
              "w_out", "ln1_g", "ln1_b", "router_group_w", "router_group_b", "router_expert_w", "router_expert_b",
              "ln2_g", "ln2_b"):
        m[n] = f32(np.asarray(inputs[n])[0])
    for n in ("expert_w_gate", "expert_w_up", "expert_w_down"):
        a = np.asarray(inputs[n])[0]
        m[n] = f32(a.reshape((NG * NE,) + a.shape[2:]))
    return m


def kernel(**inputs):
    if "nc" not in _CACHE:
        _CACHE["nc"] = build()[0]
    nc = _CACHE["nc"]
    nb = np.asarray(inputs["x"]).shape[0]
    shared = _core_inputs(inputs, 0)
    in_maps = []
    for b in range(nb):
        m = dict(shared)
        m["x"] = np.ascontiguousarray(np.asarray(inputs["x"][b], dtype=np.float32))
        in_maps.append(m)
    res = run_bass_kernel_spmd(nc, in_maps, core_ids=list(range(nb)))
    return np.stack([np.asarray(r["out"], dtype=np.float32) for r in res.results], axis=0)
```

```python
import numpy as np
from contextlib import ExitStack
import concourse.bass as bass
import concourse.mybir as mybir
from concourse.bass_utils import run_bass_kernel_spmd

F32 = mybir.dt.float32
BF16 = mybir.dt.bfloat16
AF = mybir.ActivationFunctionType
ALU = mybir.AluOpType

D = 1024
SEQ = 4096
NMETA = 16
L = SEQ + NMETA
IN_COLS = 5640
C_DNQKV, C_DNZ, C_BA, C_SBQ, C_SBK, C_SBV, C_GATE = 0, 1536, 2048, 2056, 2568, 3080, 3592
ALPHA = 2.0 ** 0.25
LN_EPS = 1e-5
RMS_EPS = 1e-6
NG, NE, FF = 4, 8, 256


class Region:
    __slots__ = ("name", "last_write", "readers", "psum", "t")

    def __init__(self, name="", psum=False):
        self.name = name
        self.last_write = None
        self.readers = []
        self.psum = psum
        self.t = 0.0


class Op:
    __slots__ = ("eng", "fn", "reads", "writes", "dma", "signal", "sem", "val", "waits", "idx", "cost")


class T:
    def __init__(self, t, name):
        self.t = t
        self.r = Region(name)

    def __getitem__(self, idx):
        return self.t[idx]


def _reg(x):
    return x.r if isinstance(x, T) else x


class Prog:
    ENGS = ("pe", "act", "dve", "pool", "sp")

    def __init__(self, nc, n_dma_sems=20):
        self.nc = nc
        self.ops = []
        self.n_dma_sems = n_dma_sems
        self.bar_regions = {e: Region("bar_" + e) for e in self.ENGS}
        self.live = {}

    def op(self, eng, fn, reads=(), writes=(), dma=False, cost=0.3):
        o = Op()
        o.cost = cost
        o.eng = eng
        o.fn = fn
        o.reads = tuple(_reg(r) for r in reads)
        o.writes = tuple(_reg(w) for w in writes)
        for r in o.reads + o.writes:
            self.live[id(r)] = r
        o.dma = dma
        o.signal = False
        o.sem = None
        o.val = 0
        o.waits = []
        o.idx = len(self.ops)
        self.ops.append(o)
        return o

    def barrier(self):
        live = list(self.live.values())
        self.op("sp", lambda e: e.nop(), reads=live, writes=live + [self.bar_regions["sp"]])
        for e in ("pe", "act", "dve", "pool"):
            self.op(e, lambda h: h.nop(), reads=[self.bar_regions["sp"]], writes=[self.bar_regions[e]])
        self.live = {}

    @staticmethod
    def _n(ap):
        n = 1
        for d in ap.shape[1:]:
            n *= int(d)
        return n

    def _ecost(self, eng, ap):
        n = self._n(ap)
        if eng == "act":
            return 0.13 + n / 1400.0
        if eng == "pool":
            return 0.15 + n * 0.0022
        return 0.08 + n / 960.0

    def dma(self, out, in_, reads=(), writes=(), eng="sp", **kw):
        return self.op(eng, lambda e: e.dma_start(out=out, in_=in_, **kw), reads, writes, dma=True, cost=2.0)

    def mm(self, out, lhsT, rhs, start=True, stop=True, reads=(), writes=()):
        return self.op("pe", lambda e: e.matmul(out, lhsT, rhs, start=start, stop=stop), reads, writes,
                       cost=0.05 + self._n(out) * 0.00056)

    def tr(self, out, in_, ident, reads=(), writes=()):
        return self.op("pe", lambda e: e.transpose(out, in_, ident), reads, writes, cost=0.15)

    def act(self, out, in_, func, reads=(), writes=(), **kw):
        return self.op("act", lambda e: e.activation(out=out, in_=in_, func=func, **kw), reads, writes,
                       cost=self._ecost("act", out))

    def tt(self, eng, out, in0, in1, op, reads=(), writes=()):
        return self.op(eng, lambda e: e.tensor_tensor(out=out, in0=in0, in1=in1, op=op), reads, writes,
                       cost=self._ecost(eng, out))

    def ts(self, eng, out, in0, s1, s2, op0, op1=None, reads=(), writes=()):
        if op1 is None:
            return self.op(eng, lambda e: e.tensor_scalar(out=out, in0=in0, scalar1=s1, scalar2=None, op0=op0),
                           reads, writes, cost=self._ecost(eng, out))
        return self.op(eng, lambda e: e.tensor_scalar(out=out, in0=in0, scalar1=s1, scalar2=s2, op0=op0, op1=op1),
                       reads, writes, cost=self._ecost(eng, out))

    def stt(self, out, in0, scalar, in1, op0, op1, reads=(), writes=()):
        return self.op("dve", lambda e: e.scalar_tensor_tensor(out=out, in0=in0, scalar=scalar, in1=in1,
                                                                op0=op0, op1=op1), reads, writes,
                       cost=self._ecost("dve", out))

    def copy(self, eng, out, in_, reads=(), writes=()):
        if eng == "act":
            return self.op("act", lambda e: e.copy(out=out, in_=in_), reads, writes, cost=self._ecost("act", out))
        return self.op(eng, lambda e: e.tensor_copy(out=out, in_=in_), reads, writes, cost=self._ecost(eng, out))

    def sim_new_ops(self, n0):
        if not hasattr(self, "eng_t"):
            self.eng_t = {e: 0.0 for e in self.ENGS}
        fin_max = 0.0
        for o in self.ops[n0:]:
            start = self.eng_t[o.eng]
            for r in o.reads + o.writes:
                if r.t + 0.15 > start:
                    start = r.t + 0.15
            fin = start + o.cost
            if not o.dma:
                self.eng_t[o.eng] = fin
            else:
                self.eng_t[o.eng] = start + 0.05
            for r in o.writes:
                r.t = fin
            for r in o.reads:
                if o.dma or r.psum:
                    r.t = max(r.t, fin)
            fin_max = max(fin_max, fin)
        return fin_max

    def run_sched(self, queue, slotsets=None, prestarted=()):
        slotsets = slotsets or {}
        free = {k: list(range(len(v))) for k, v in slotsets.items()}
        active = [[g, None, None, 0.0, None] for g in prestarted]
        queue = list(queue)
        done = set()

        def startable(it):
            if it == "drain":
                return False
            if it[0] is not None and not free[it[0]]:
                return False
            return all(a in done for a in (it[3] if len(it) > 3 else ()))

        while queue or active:
            while queue and startable(queue[0]):
                it = queue.pop(0)
                cls, mk = it[0], it[1]
                name = it[2] if len(it) > 2 else None
                t_now = min([a[3] for a in active], default=0.0)
                if cls is None:
                    active.append([mk(None), None, None, t_now, name])
                else:
                    si = free[cls].pop(0)
                    active.append([mk(slotsets[cls][si]), cls, si, t_now, name])
            if queue and queue[0] == "drain" and not active:
                queue.pop(0)
                continue
            assert active, "scheduler deadlock: head of queue waits for a generator that was never started"
            item = min(active, key=lambda a: a[3])
            n0 = len(self.ops)
            try:
                next(item[0])
                fin = self.sim_new_ops(n0)
                item[3] = max(item[3], fin) if fin > 0 else item[3] + 0.01
            except StopIteration:
                self.sim_new_ops(n0)
                active.remove(item)
                if item[1] is not None:
                    free[item[1]].append(item[2])
                if item[4] is not None:
                    done.add(item[4])


    def finalize(self, stack):
        nc = self.nc
        ops = self.ops
        deps_of = []
        for o in ops:
            deps = {}
            for r in o.reads:
                if r.last_write is not None:
                    deps[r.last_write] = "raw"
                if r.psum:
                    for rd in r.readers:
                        if ops[rd].eng != o.eng and rd not in deps:
                            deps[rd] = "rr"
            for w in o.writes:
                if w.last_write is not None and w.last_write not in deps:
                    deps[w.last_write] = "waw"
                for rd in w.readers:
                    if rd not in deps:
                        deps[rd] = "war"
            for r in o.reads:
                if o.dma:
                    r.readers.append(o.idx)
                else:
                    r.readers = [q for q in r.readers if ops[q].dma or ops[q].eng != o.eng]
                    r.readers.append(o.idx)
            for w in o.writes:
                w.last_write = o.idx
                w.readers = []
            keep = []
            for j, kind in deps.items():
                if j == o.idx:
                    continue
                p = ops[j]
                if p.eng == o.eng and not p.dma and not o.dma:
                    if o.eng == "pe" or kind == "rr":
                        continue
                keep.append(j)
            deps_of.append(keep)
            for j in keep:
                ops[j].signal = True
        eng_sem = {e: stack.enter_context(nc.semaphore("s_" + e)) for e in self.ENGS}
        dma_engs = ("sp", "pool", "act")
        dma_pool = {e: [stack.enter_context(nc.semaphore("d_%s%d" % (e, i))) for i in range(self.n_dma_sems)]
                    for e in dma_engs}
        dma_val = {e: [0] * self.n_dma_sems for e in dma_engs}
        dma_rr = {e: 0 for e in dma_engs}
        eng_cnt = {e: 0 for e in self.ENGS}
        for o in ops:
            pre = []
            if o.dma:
                k = dma_rr[o.eng]
                dma_rr[o.eng] = (k + 1) % self.n_dma_sems
                if dma_val[o.eng][k] > 0:
                    pre.append((dma_pool[o.eng][k], dma_val[o.eng][k]))
                dma_val[o.eng][k] += 16
                o.sem = dma_pool[o.eng][k]
                o.val = dma_val[o.eng][k]
                o.signal = True
            elif o.signal:
                eng_cnt[o.eng] += 1
                o.sem = eng_sem[o.eng]
                o.val = eng_cnt[o.eng]
            best = {}
            for (sem, val) in pre + [(ops[j].sem, ops[j].val) for j in deps_of[o.idx]]:
                key = id(sem)
                if key not in best or best[key][1] < val:
                    best[key] = (sem, val)
            o.waits = list(best.values())
        self.stats = {e: sum(1 for o in ops if o.eng == e) for e in self.ENGS}
        self.stats["signals"] = dict(eng_cnt)
        per_eng = {e: [o for o in ops if o.eng == e] for e in self.ENGS}
        block = stack.enter_context(nc.Block())
        nds = self.n_dma_sems

        def emit_stream(e, handle):
            known = {}
            nw = 0
            for o in per_eng[e]:
                for (sem, val) in o.waits:
                    key = id(sem)
                    if known.get(key, 0) >= val:
                        continue
                    known[key] = val
                    handle.wait_ge(sem, val)
                    nw += 1
                ins = o.fn(handle)
                if o.signal:
                    ins.then_inc(o.sem, 16 if o.dma else 1)
            if e in dma_pool:
                for k in range(nds):
                    if dma_val[e][k] > 0 and known.get(id(dma_pool[e][k]), 0) < dma_val[e][k]:
                        handle.wait_ge(dma_pool[e][k], dma_val[e][k])
            self.stats["waits_" + e] = nw

        @block.tensor
        def _(h):
            emit_stream("pe", h)

        @block.scalar
        def _(h):
            emit_stream("act", h)

        @block.vector
        def _(h):
            emit_stream("dve", h)

        @block.gpsimd
        def _(h):
            emit_stream("pool", h)

        @block.sync
        def _(h):
            emit_stream("sp", h)


def rsqrt(P, out, in_, eps, reads, wt):
    P.act(out, in_, AF.Ln, reads=reads, writes=[wt], bias=eps)
    P.act(out, out, AF.Exp, reads=[wt], writes=[wt], scale=-0.5)


class Rot:
    def __init__(self, items):
        self.items = items
        self.i = 0

    def next(self):
        x = self.items[self.i % len(self.items)]
        self.i += 1
        return x


def st_range(j):
    if j == 0:
        return 0, NMETA
    return NMETA + 512 * (j - 1), 512


N_ST = 9


class Ctx:
    pass


def build(debug=(), phases=(1, 2, 3, 4, 5), scratch_in=()):
    nc = bass.Bass("TRN2", target_bir_lowering=False)
    c = Ctx()
    c.nc = nc
    c.debug = debug
    c.phases = phases

    def din(name, shape):
        return nc.dram_tensor(name, list(shape), F32, kind="ExternalInput").ap()

    c.x = din("x", (SEQ, D))
    c.meta = din("meta_tokens", (NMETA, D))
    c.ln_emb_g = din("ln_emb_g", (D,))
    c.ln_emb_b = din("ln_emb_b", (D,))
    c.w_in = din("w_in", (D, IN_COLS))
    c.b_gate = din("b_gate", (2, D))
    c.conv_w = din("dn_conv_w", (4, 1536))
    c.a_log = din("dn_a_log", (4,))
    c.dt_bias = din("dn_dt_bias", (4,))
    c.norm_g = din("dn_norm_g", (128,))
    c.w_bdn = din("w_branch_dn", (512, D))
    c.w_bsb = din("w_branch_sb", (512, D))
    c.w_out = din("w_out", (D, D))
    c.ln1_g = din("ln1_g", (D,))
    c.ln1_b = din("ln1_b", (D,))
    c.rg_w = din("router_group_w", (D, NG))
    c.rg_b = din("router_group_b", (NG,))
    c.re_w = din("router_expert_w", (NG, D, NE))
    c.re_b = din("router_expert_b", (NG, NE))
    c.e_gate = din("expert_w_gate", (NG * NE, D, FF))
    c.e_up = din("expert_w_up", (NG * NE, D, FF))
    c.e_down = din("expert_w_down", (NG * NE, FF, D))
    c.ln2_g = din("ln2_g", (D,))
    c.ln2_b = din("ln2_b", (D,))
    c.out = nc.dram_tensor("out", [SEQ, D], F32, kind="ExternalOutput").ap()

    def scratch(name, shape, dt):
        kind = "ExternalOutput" if name in debug else ("ExternalInput" if name in scratch_in else "Internal")
        return T(nc.dram_tensor(name, list(shape), dt, kind=kind).ap(), name)

    c.h0s = scratch("h0s", (L, D), F32)
    c.qT_dn = scratch("qT_dn", (4, 128, L), BF16)
    c.kT_dn = scratch("kT_dn", (4, 128, L), BF16)
    c.k_dn = scratch("k_dn", (L, 512), BF16)
    c.v_dn = scratch("v_dn", (L, 512), BF16)
    c.bgs = scratch("bgs", (L, 8), F32)
    c.zsT = scratch("zsT", (4, 128, L), BF16)
    c.qT_sb = scratch("qT_sb", (4, 128, L), BF16)
    c.kT_sb = scratch("kT_sb", (4, 128, L), BF16)
    c.v_sb = scratch("v_sb", (L, 512), BF16)
    c.gsT = scratch("gsT", (16, 128, L), BF16)
    c.odnT = scratch("odnT", (4, 128, L), BF16)
    c.osbT = scratch("osbT", (4, 128, L), BF16)
    c.h1s = scratch("h1s", (SEQ, D), F32)
    c.h1T = scratch("h1T", (8, 128, SEQ), BF16)
    c.combs = scratch("combs", (SEQ, 32), F32)

    with ExitStack() as st:
        P = Prog(nc)
        c.P = P
        c.st = st
        phase_consts(c)
        if 1 in c.phases:
            phase1(c)
        if 2 in c.phases:
            phase2(c)
        if 3 in c.phases:
            phase3(c)
        if 4 in c.phases:
            phase4(c)
        if 5 in c.phases:
            phase5(c)
        P.finalize(st)
        c.stats = P.stats
    return nc, c


def sb(c, st, name, shape, dt):
    return T(st.enter_context(c.nc.sbuf_tensor(name, list(shape), dt)), name)


def ps(c, st, name, shape, dt=F32):
    nbytes = int(np.prod(shape[1:])) * (4 if dt == F32 else 2)
    assert nbytes % 2048 == 0, (name, shape)
    t = T(st.enter_context(c.nc.psum_tensor(name, list(shape), dt)), name)
    t.r.psum = True
    return t


def phase_consts(c):
    P, st, nc = c.P, c.st, c.nc
    c.ident = sb(c, st, "ident", (128, 128), F32)
    c.identb = sb(c, st, "identb", (128, 128), BF16)
    c.onesb = sb(c, st, "onesb", (128, 128), BF16)
    P.op("pool", lambda e: e.memset(c.ident[:], 0.0), writes=[c.ident])
    P.op("pool", lambda e: e.affine_select(out=c.ident[:], in_=c.ident[:], pattern=[[-1, 128]],
                                           compare_op=ALU.not_equal, fill=1.0, base=0, channel_multiplier=1),
         reads=[c.ident], writes=[c.ident])
    P.copy("pool", c.identb[:], c.ident[:], reads=[c.ident], writes=[c.identb])
    P.op("pool", lambda e: e.memset(c.onesb[:], 1.0), writes=[c.onesb])
    c.ident4 = sb(c, st, "ident4", (128, 4, 128), F32)
    for h in range(4):
        P.copy("pool", c.ident4[:, h, :], c.ident[:], reads=[c.ident], writes=[c.ident4])


def run_window(P, queue, slots, G):
    free = list(range(len(slots)))
    active = []
    queue = list(queue)
    while queue or active:
        while queue and queue[0] != "drain" and len(active) < G and free:
            mk = queue.pop(0)
            si = free.pop(0)
            active.append((mk(slots[si]), si))
        if queue and queue[0] == "drain" and not active:
            queue.pop(0)
            continue
        for item in list(active):
            try:
                next(item[0])
            except StopIteration:
                active.remove(item)
                free.append(item[1])


def phase1(c):
    P, nc = c.P, c.nc
    NSLOT = 7
    with ExitStack() as st:
        w_in = sb(c, st, "w_in_sb", (128, 8, IN_COLS), BF16)
        gbc = sb(c, st, "gbc", (128, D), F32)
        bbc = sb(c, st, "bbc", (128, D), F32)
        cw = sb(c, st, "cw", (128, 12, 4), F32)
        cwraw = sb(c, st, "cwraw", (4, 1536), F32)
        bgT = sb(c, st, "bgT", (128, 16), F32)
        bgraw = sb(c, st, "bgraw", (16, 128), F32)
        alog = sb(c, st, "alog", (128, 4), F32)
        negA = sb(c, st, "negA", (128, 4), F32)
        dtb = sb(c, st, "dtb", (128, 4), F32)
        hist = sb(c, st, "hist", (128, 12, 3), F32)
        h0Ts = [sb(c, st, "h0T%d" % i, (128, 8, 512), BF16) for i in range(2)]
        xb = Rot([sb(c, st, "xb%d" % i, (128, D), F32) for i in range(4)])
        hb = Rot([sb(c, st, "hb%d" % i, (128, D), F32) for i in range(4)])
        stats = Rot([sb(c, st, "stats%d" % i, (128, 16), F32) for i in range(4)])
        lnslots = [{} for i in range(4)]
        slots = []
        for i in range(NSLOT):
            slots.append({"pc": sb(c, st, "pc%d" % i, (128, 515), F32), "acc": sb(c, st, "acc%d" % i, (128, 512), F32),
                          "sq": sb(c, st, "sq%d" % i, (128, 512), BF16), "o": sb(c, st, "o%d" % i, (128, 512), BF16),
                          "tk": sb(c, st, "tk%d" % i, (128, 512), BF16), "bg": sb(c, st, "bgb%d" % i, (128, 16), F32)})
        pT = ps(c, st, "pT", (128, D), F32)
        pmm = Rot([ps(c, st, "pmm%d" % i, (128, 512), F32) for i in range(4)])
        ptr = Rot([ps(c, st, "ptr%d" % i, (128, 1024), BF16) for i in range(2)])

        CG = ((0, 1536), (1536, 3080), (3080, 4360), (4360, IN_COLS))
        w_in_rr = [[Region("w_in_%d_%d" % (k, g)) for g in range(4)] for k in range(8)]
        for g, (c0, c1) in enumerate(CG):
            for k in range(8):
                P.dma(w_in[:, k, c0:c1], c.w_in[k * 128:(k + 1) * 128, c0:c1], writes=[w_in_rr[k][g]], eng="pool")

        def wreg(k, c0, width=128):
            return [w_in_rr[k][g] for g, (a0, a1) in enumerate(CG) if c0 < a1 and c0 + width > a0]
        P.dma(gbc[:], c.ln_emb_g.partition_broadcast(128), writes=[gbc])
        P.dma(bbc[:], c.ln_emb_b.partition_broadcast(128), writes=[bbc])
        P.dma(cwraw[:], c.conv_w, writes=[cwraw])
        P.dma(bgraw[:], c.b_gate.rearrange("a (c p) -> (a c) p", p=128), writes=[bgraw])
        P.dma(alog[:], c.a_log.partition_broadcast(128), writes=[alog])
        P.dma(dtb[:], c.dt_bias.partition_broadcast(128), writes=[dtb])
        P.op("pool", lambda e: e.memset(hist[:], 0.0), writes=[hist])
        for cc in range(12):
            pp = pmm.next()
            P.tr(pp[:, 0:4], cwraw[0:4, cc * 128:(cc + 1) * 128], c.ident[0:4, 0:4], reads=[cwraw, c.ident],
                 writes=[pp])
            P.copy("dve", cw[:, cc, :], pp[:, 0:4], reads=[pp], writes=[cw])
        pp = pmm.next()
        P.tr(pp[:, 0:16], bgraw[0:16, :], c.ident[0:16, 0:16], reads=[bgraw, c.ident], writes=[pp])
        P.copy("dve", bgT[:], pp[:, 0:16], reads=[pp], writes=[bgT])
        P.act(negA[:], alog[:], AF.Exp, reads=[alog], writes=[negA])
        P.ts("dve", negA[:], negA[:], -1.0, None, ALU.mult, reads=[negA], writes=[negA])

        def gen_ln(j, s, slot):
            t0, Tn = st_range(j)
            h0T = h0Ts[j % 2]
            rows = min(128, Tn)
            r0 = t0 + s * 128
            xt, ht, stt_ = xb.next(), hb.next(), stats.next()
            if j == 0:
                P.dma(xt[:rows, :], c.meta[:, :], writes=[xt])
            else:
                P.dma(xt[:rows, :], c.x[r0 - NMETA:r0 - NMETA + rows, :], writes=[xt])
            yield
            P.op("dve", lambda e: e.bn_stats(out=stt_[:rows, 0:6], in_=xt[:rows, 0:512]), reads=[xt], writes=[stt_],
                 cost=0.7)
            P.op("dve", lambda e: e.bn_stats(out=stt_[:rows, 6:12], in_=xt[:rows, 512:1024]), reads=[xt],
                 writes=[stt_], cost=0.7)
            P.op("dve", lambda e: e.bn_aggr(out=stt_[:rows, 12:14], in_=stt_[:rows, 0:12]), reads=[stt_],
                 writes=[stt_])
            P.act(stt_[:rows, 14:15], stt_[:rows, 13:14], AF.Ln, reads=[stt_], writes=[stt_], bias=LN_EPS)
            yield
            P.act(stt_[:rows, 14:15], stt_[:rows, 14:15], AF.Exp, reads=[stt_], writes=[stt_], scale=-0.5)
            P.stt(stt_[:rows, 15:16], stt_[:rows, 12:13], -1.0, stt_[:rows, 14:15], ALU.mult, ALU.mult, reads=[stt_],
                  writes=[stt_])
            yield
            P.act(xt[:rows, :], xt[:rows, :], AF.Identity, reads=[xt, stt_], writes=[xt], scale=stt_[:rows, 14:15],
                  bias=stt_[:rows, 15:16])
            yield
            P.tt("dve", xt[:rows, :], xt[:rows, :], gbc[:rows, :], ALU.mult, reads=[xt, gbc], writes=[xt])
            yield
            P.tt("pool", ht[:rows, :], xt[:rows, :], bbc[:rows, :], ALU.add, reads=[xt, bbc], writes=[ht])
            P.dma(c.h0s[r0:r0 + rows, :], ht[:rows, :], reads=[ht], writes=[Region()])
            yield
            for k in range(8):
                P.tr(pT[:, k * 128:k * 128 + rows], ht[:rows, k * 128:(k + 1) * 128], c.ident[:rows, :rows],
                     reads=[ht, c.ident], writes=[pT])
            P.copy("act", h0T[:, :, s * 128:s * 128 + rows],
                   pT[:].rearrange("p (k t) -> p k t", k=8)[:, :, 0:rows], reads=[pT], writes=[h0T])
            yield

        def proj(c0, j):
            t0, Tn = st_range(j)
            h0T = h0Ts[j % 2]
            pp = pmm.next()
            for k in range(8):
                P.mm(pp[:, :Tn], w_in[:, k, c0:c0 + 128], h0T[:, k, :Tn], start=(k == 0), stop=(k == 7),
                     reads=wreg(k, c0) + [h0T], writes=[pp])
            return pp

        def to_tok(j, o, tk, dst, h):
            t0, Tn = st_range(j)
            nsub = max(1, Tn // 128)
            rows = min(128, Tn)
            pt = ptr.next()
            for s in range(nsub):
                P.tr(pt[:rows, s * 128:(s + 1) * 128], o[:, s * 128:s * 128 + rows], c.identb[:, :],
                     reads=[o, c.identb], writes=[pt])
            P.copy("dve", tk[:rows, 0:nsub * 128], pt[:rows, 0:nsub * 128], reads=[pt], writes=[tk])
            for s in range(nsub):
                P.dma(dst[t0 + s * 128:t0 + s * 128 + rows, h * 128:(h + 1) * 128], tk[:rows, s * 128:(s + 1) * 128],
                      reads=[tk], writes=[Region()])

        def gen_dn(j, cc, slot):
            t0, Tn = st_range(j)
            kind, h = cc // 4, cc % 4
            pc, acc, sq, o, tk = slot["pc"], slot["acc"], slot["sq"], slot["o"], slot["tk"]
            pp = proj(C_DNQKV + cc * 128, j)
            P.copy("act", pc[:, 3:3 + Tn], pp[:, :Tn], reads=[pp], writes=[pc])
            P.copy("pool", pc[:, 0:3], hist[:, cc, :], reads=[hist], writes=[pc])
            yield
            P.ts("dve", acc[:, :Tn], pc[:, 3:3 + Tn], cw[:, cc, 3:4], None, ALU.mult, reads=[pc, cw], writes=[acc])
            for tap in (2, 1, 0):
                P.stt(acc[:, :Tn], pc[:, tap:tap + Tn], cw[:, cc, tap:tap + 1], acc[:, :Tn], ALU.mult, ALU.add,
                      reads=[pc, cw, acc], writes=[acc])
            P.copy("pool", hist[:, cc, :], pc[:, Tn:Tn + 3], reads=[pc], writes=[hist])
            yield
            if kind == 2:
                P.act(o[:, :Tn], acc[:, :Tn], AF.Silu, reads=[acc], writes=[o])
                yield
                to_tok(j, o, tk, c.v_dn, h)
                return
            sl = pc
            P.act(sl[:, 3:3 + Tn], acc[:, :Tn], AF.Silu, reads=[acc], writes=[pc])
            yield
            P.tt("dve", sq[:, :Tn], sl[:, 3:3 + Tn], sl[:, 3:3 + Tn], ALU.mult, reads=[pc], writes=[sq])
            yield
            p2 = pmm.next()
            P.mm(p2[:, :Tn], c.onesb[:, :], sq[:, :Tn], reads=[c.onesb, sq], writes=[p2])
            P.act(acc[:, :Tn], p2[:, :Tn], AF.Ln, reads=[p2], writes=[acc], bias=RMS_EPS)
            yield
            P.act(acc[:, :Tn], acc[:, :Tn], AF.Exp, reads=[acc], writes=[acc], scale=-0.5)
            if kind == 0:
                P.stt(o[:, :Tn], sl[:, 3:3 + Tn], 128.0 ** -0.5, acc[:, :Tn], ALU.mult, ALU.mult,
                      reads=[pc, acc], writes=[o])
                P.dma(c.qT_dn[h, :, t0:t0 + Tn], o[:, :Tn], reads=[o], writes=[Region()])
            else:
                P.tt("dve", o[:, :Tn], sl[:, 3:3 + Tn], acc[:, :Tn], ALU.mult, reads=[pc, acc], writes=[o])
                P.dma(c.kT_dn[h, :, t0:t0 + Tn], o[:, :Tn], reads=[o], writes=[Region()])
                yield
                to_tok(j, o, tk, c.k_dn, h)

        def gen_simple(j, kind, cc, slot):
            t0, Tn = st_range(j)
            o = slot["o"]
            if kind == "z":
                pp = proj(C_DNZ + cc * 128, j)
                P.act(o[:, :Tn], pp[:, :Tn], AF.Silu, reads=[pp], writes=[o])
                P.dma(c.zsT[cc, :, t0:t0 + Tn], o[:, :Tn], reads=[o], writes=[Region()])
            elif kind == "sbq":
                pp = proj(C_SBQ + cc * 128, j)
                P.ts("dve", o[:, :Tn], pp[:, :Tn], 0.125, None, ALU.mult, reads=[pp], writes=[o])
                P.dma(c.qT_sb[cc, :, t0:t0 + Tn], o[:, :Tn], reads=[o], writes=[Region()])
            elif kind == "sbk":
                pp = proj(C_SBK + cc * 128, j)
                P.copy("dve", o[:, :Tn], pp[:, :Tn], reads=[pp], writes=[o])
                P.dma(c.kT_sb[cc, :, t0:t0 + Tn], o[:, :Tn], reads=[o], writes=[Region()])
            else:
                pp = proj(C_GATE + cc * 128, j)
                P.act(o[:, :Tn], pp[:, :Tn], AF.Sigmoid, reads=[pp, bgT], writes=[o], bias=bgT[:, cc:cc + 1])
                P.dma(c.gsT[cc, :, t0:t0 + Tn], o[:, :Tn], reads=[o], writes=[Region()])
            yield

        def gen_tok(j, s, slot):
            t0, Tn = st_range(j)
            h0T = h0Ts[j % 2]
            rows = min(128, Tn)
            r0 = t0 + s * 128
            tk, bg = slot["tk"], slot["bg"]
            pp = pmm.next()
            for k in range(8):
                P.mm(pp[:rows, 0:512], h0T[:, k, s * 128:s * 128 + rows], w_in[:, k, C_SBV:C_SBV + 512],
                     start=(k == 0), stop=(k == 7), reads=wreg(k, C_SBV, 512) + [h0T], writes=[pp])
            P.copy("act", tk[:rows, :], pp[:rows, :], reads=[pp], writes=[tk])
            P.dma(c.v_sb[r0:r0 + rows, :], tk[:rows, :], reads=[tk], writes=[Region()])
            yield
            pp = pmm.next()
            for k in range(8):
                P.mm(pp[:rows, 0:8], h0T[:, k, s * 128:s * 128 + rows], w_in[:, k, C_BA:C_BA + 8],
                     start=(k == 0), stop=(k == 7), reads=wreg(k, C_BA, 8) + [h0T], writes=[pp])
            P.act(bg[:rows, 0:4], pp[:rows, 0:4], AF.Sigmoid, reads=[pp], writes=[bg])
            P.tt("dve", bg[:rows, 8:12], pp[:rows, 4:8], dtb[:rows, :], ALU.add, reads=[pp, dtb], writes=[bg])
            yield
            P.act(bg[:rows, 8:12], bg[:rows, 8:12], AF.Exp, reads=[bg], writes=[bg])
            P.act(bg[:rows, 8:12], bg[:rows, 8:12], AF.Ln, reads=[bg], writes=[bg], bias=1.0)
            P.tt("dve", bg[:rows, 4:8], bg[:rows, 8:12], negA[:rows, :], ALU.mult, reads=[bg, negA], writes=[bg])
            P.dma(c.bgs[r0:r0 + rows, :], bg[:rows, 0:8], reads=[bg], writes=[Region()])
            yield

        def nsub_of(j):
            return max(1, st_range(j)[1] // 128)

        def ln_items(j, after):
            return [("ln", (lambda sl_, j=j, s_=s_: gen_ln(j, s_, sl_)), "ln%d_%d" % (j, s_), tuple(after))
                    for s_ in range(nsub_of(j))]

        queue = ln_items(0, ())
        names = {}
        for j in range(N_ST):
            nsub = nsub_of(j)
            items = []
            names[j] = []

            def add(mk, name, after, j=j):
                items.append(("s", mk, name, tuple(after)))
                names[j].append(name)

            base = ["ln%d_%d" % (j, s_) for s_ in range(nsub)]
            for cc in range(12):
                add((lambda sl_, j=j, cc=cc: gen_dn(j, cc, sl_)), "dn%d_%d" % (j, cc),
                    base + (["dn%d_%d" % (j - 1, cc)] if j > 0 else []))
                if cc == 3 and j + 1 < N_ST:
                    items.extend(ln_items(j + 1, names[j - 1] if j > 0 else ()))
            for cc in range(4):
                add((lambda sl_, j=j, cc=cc: gen_simple(j, "z", cc, sl_)), "z%d_%d" % (j, cc), base)
            for s_ in range(nsub):
                add((lambda sl_, j=j, s_=s_: gen_tok(j, s_, sl_)), "tok%d_%d" % (j, s_), base)
            for cc in range(4):
                add((lambda sl_, j=j, cc=cc: gen_simple(j, "sbq", cc, sl_)), "sbq%d_%d" % (j, cc), base)
                add((lambda sl_, j=j, cc=cc: gen_simple(j, "sbk", cc, sl_)), "sbk%d_%d" % (j, cc), base)
            for cc in range(16):
                add((lambda sl_, j=j, cc=cc: gen_simple(j, "gate", cc, sl_)), "gate%d_%d" % (j, cc), base)
            queue += items
        P.run_sched(queue, {"s": slots, "ln": lnslots})
        P.barrier()


def chunk_range(ci):
    if ci == 0:
        return 0, NMETA
    return NMETA + 128 * (ci - 1), 128


N_CH = 33


def phase2(c):
    P, nc = c.P, c.nc
    GRP = 3
    NSLOT = 2 * GRP
    with ExitStack() as st:
        def t32(name):
            return sb(c, st, name, (128, 4, 128), F32)

        def t16(name):
            return sb(c, st, name, (128, 4, 128), BF16)

        Mst, Min, M32, M64, Mc32, Mc64 = t32("Mst"), t32("Min"), t32("M32"), t32("M64"), t32("Mc32"), t32("Mc64")
        ones32 = sb(c, st, "ones32", (128, 128), F32)
        normg = sb(c, st, "normg", (128, 1), F32)
        S = t32("S")
        Sb = t16("Sb")
        slots = []
        inner = []
        for g in range(GRP):
            d = {}
            for n in ("N", "NT", "Nd0", "NdT0", "Nd1", "NdT1", "C32", "C32T", "C64T", "Zq0", "Zq1", "X", "XT",
                      "U", "Up", "X2", "XT2"):
                d[n] = t16("%s_%d" % (n, g))
            for n in ("Z", "g_", "E", "es", "ei", "ed"):
                d[n] = t32("%s_%d" % (n, g))
            inner.append(d)
        for g in range(NSLOT):
            d = dict(inner[g % GRP])
            for n in ("qT", "kT", "kt", "vt", "zs", "Qd", "Kd", "PT", "X3"):
                d[n] = t16("%s_%d" % (n, g))
            d["bg"] = sb(c, st, "bg_%d" % g, (128, 8), F32)
            d["sm"] = sb(c, st, "sm_%d" % g, (128, 32), F32)
            slots.append(d)
        rb = Rot([t16("rb%d" % i) for i in range(2)])
        vn = Rot([t16("vn%d" % i) for i in range(2)])
        osb = Rot([t32("osb%d" % i) for i in range(2)])
        osq = Rot([t16("osq%d" % i) for i in range(2)])
        ors = Rot([t32("ors%d" % i) for i in range(2)])
        oo = Rot([t16("oo%d" % i) for i in range(2)])
        pp = Rot([ps(c, st, "p2_%d" % i, (128, 4, 128), F32) for i in range(6)])
        ptb = Rot([ps(c, st, "p2_tb%d" % i, (128, 8, 128), BF16) for i in range(2)])

        def fill_tri(t, op):
            P.op("pool", lambda e: e.memset(t[:], 1.0), writes=[t])
            P.op("pool", lambda e: e.affine_select(out=t[:], in_=t[:], pattern=[[0, 4], [1, 128]], compare_op=op,
                                                   fill=0.0, base=0, channel_multiplier=-1), reads=[t], writes=[t])

        def fill_block(t, bs):
            P.op("pool", lambda e: e.memset(t[:], 1.0), writes=[t])
            for a in range(128 // bs):
                v = t[:, :, a * bs:(a + 1) * bs]
                P.op("pool", lambda e, v=v, a=a: e.affine_select(out=v, in_=v, pattern=[[0, 4], [0, bs]],
                                                                  compare_op=ALU.is_ge, fill=0.0, base=-a * bs,
                                                                  channel_multiplier=1), reads=[t], writes=[t])
                P.op("pool", lambda e, v=v, a=a: e.affine_select(out=v, in_=v, pattern=[[0, 4], [0, bs]],
                                                                  compare_op=ALU.is_ge, fill=0.0, base=a * bs + bs - 1,
                                                                  channel_multiplier=-1), reads=[t], writes=[t])

        fill_tri(Mst, ALU.is_gt)
        fill_tri(Min, ALU.is_ge)
        fill_block(M32, 32)
        fill_block(M64, 64)
        P.tt("pool", Mc32[:], M64[:], M32[:], ALU.subtract, reads=[M64, M32], writes=[Mc32])
        P.ts("pool", Mc64[:], M64[:], -1.0, 1.0, ALU.mult, ALU.add, reads=[M64], writes=[Mc64])
        P.op("pool", lambda e: e.memset(ones32[:], 1.0), writes=[ones32])
        P.op("pool", lambda e: e.memset(S[:], 0.0), writes=[S])
        P.op("pool", lambda e: e.memset(Sb[:], 0.0), writes=[Sb])
        P.dma(normg[:], c.norm_g.rearrange("(p o) -> p o", o=1), writes=[normg])

        def pre(ci, delay=0):
            for _ in range(delay):
                yield
            c0, C = chunk_range(ci)
            d = slots[ci % NSLOT]
            qT, kT, kt, vt, zs, bg, s_ = d["qT"], d["kT"], d["kt"], d["vt"], d["zs"], d["bg"], d["sm"]
            P.dma(qT[:, :, :C], c.qT_dn[:, :, c0:c0 + C].rearrange("h p t -> p h t"), writes=[qT])
            P.dma(kT[:, :, :C], c.kT_dn[:, :, c0:c0 + C].rearrange("h p t -> p h t"), writes=[kT])
            P.dma(kt[:C, :, :], c.k_dn[c0:c0 + C, :].rearrange("t (h d) -> t h d", h=4), writes=[kt])
            P.dma(vt[:C, :, :], c.v_dn[c0:c0 + C, :].rearrange("t (h d) -> t h d", h=4), writes=[vt])
            P.dma(zs[:, :, :C], c.zsT[:, :, c0:c0 + C].rearrange("h p t -> p h t"), writes=[zs])
            P.dma(bg[:C, :], c.bgs[c0:c0 + C, :], writes=[bg])
            yield
            g_, E, es, ei, ed = d["g_"], d["E"], d["es"], d["ei"], d["ed"]
            for h in range(4):
                P.act(g_[:C, h, :], ones32[:C, :], AF.Copy, reads=[ones32, bg], writes=[g_], scale=bg[:C, 4 + h:5 + h])
            pd = pp.next()
            for h in range(4):
                P.mm(pd[:, h, :C], g_[:C, h, :], Min[:C, 0, :C], reads=[g_, Min], writes=[pd])
            pc_ = pp.next()
            P.mm(pc_[:C, 0, 0:4], Min[:C, 0, :C], bg[:C, 4:8], reads=[Min, bg], writes=[pc_])
            P.copy("dve", s_[:C, 0:4], pc_[:C, 0, 0:4], reads=[pc_], writes=[s_])
            P.ts("dve", s_[:C, 4:8], pc_[:C, 0, 0:4], -1.0, None, ALU.mult, reads=[pc_], writes=[s_])
            P.act(s_[:C, 8:12], s_[:C, 0:4], AF.Exp, reads=[s_], writes=[s_])
            P.ts("dve", s_[:C, 8:12], s_[:C, 8:12], -1.0, None, ALU.mult, reads=[s_], writes=[s_])
            P.tt("dve", s_[:C, 12:16], pd[:C, :, C - 1], s_[:C, 0:4], ALU.subtract, reads=[pd, s_], writes=[s_])
            P.act(s_[:C, 12:16], s_[:C, 12:16], AF.Exp, reads=[s_], writes=[s_])
            P.act(s_[:, 16:20], pd[:, :, C - 1], AF.Exp, reads=[pd], writes=[s_])
            for h in range(4):
                P.ts("dve", E[:C, h, :C], pd[:C, h, :C], s_[:C, 4 + h:5 + h], 0.0, ALU.add, ALU.min,
                     reads=[pd, s_], writes=[E])
            P.act(ed[:, :, :C], pd[:, :, :C], AF.Exp, reads=[pd], writes=[ed])
            yield
            P.act(E[:C, :, :C], E[:C, :, :C], AF.Exp, reads=[E], writes=[E])
            P.tt("pool", es[:C, :, :C], E[:C, :, :C], Mst[:C, :, :C], ALU.mult, reads=[E, Mst], writes=[es])
            P.tt("pool", ei[:C, :, :C], E[:C, :, :C], Min[:C, :, :C], ALU.mult, reads=[E, Min], writes=[ei])
            Qd, Kd, PT = d["Qd"], d["Kd"], d["PT"]
            P.tt("pool", Qd[:, :, :C], qT[:, :, :C], ed[:, :, :C], ALU.mult, reads=[qT, ed], writes=[Qd])
            for h in range(4):
                P.act(Kd[:C, h, :], kt[:C, h, :], AF.Copy, reads=[kt, s_], writes=[Kd], scale=s_[:C, 12 + h:13 + h])
            yield
            pG = pp.next()
            for h in range(4):
                P.mm(pG[:C, h, :C], kT[:, h, :C], kT[:, h, :C], reads=[kT], writes=[pG])
            pQK = pp.next()
            for h in range(4):
                P.mm(pQK[:C, h, :C], kT[:, h, :C], qT[:, h, :C], reads=[kT, qT], writes=[pQK])
            P.tt("dve", PT[:C, :, :C], pQK[:C, :, :C], ei[:C, :, :C], ALU.mult, reads=[pQK, ei], writes=[PT])
            N, NT = d["N"], d["NT"]
            for h in range(4):
                P.stt(N[:C, h, :C], pG[:C, h, :C], bg[:C, h:h + 1], es[:C, h, :C], ALU.mult, ALU.mult,
                      reads=[pG, bg, es], writes=[N])
            yield
            pt_ = ptb.next()
            for h in range(4):
                P.tr(pt_[:C, h, :C], N[:C, h, :C], c.identb[:C, :C], reads=[N, c.identb], writes=[pt_])
            P.copy("act", NT[:C, :, :C], pt_[:C, 0:4, :C], reads=[pt_], writes=[NT])
            Nd, NdT = d["Nd0"], d["NdT0"]
            P.tt("pool", Nd[:C, :, :C], N[:C, :, :C], M32[:C, :, :C], ALU.mult, reads=[N, M32], writes=[Nd])
            P.tt("pool", d["C32"][:C, :, :C], N[:C, :, :C], Mc32[:C, :, :C], ALU.mult, reads=[N, Mc32],
                 writes=[d["C32"]])
            Z, Zq = d["Z"], d["Zq0"]
            P.tt("dve", Z[:C, :, :C], c.ident4[:C, :, :C], Nd[:C, :, :C], ALU.subtract, reads=[c.ident4, Nd],
                 writes=[Z])
            yield
            P.tt("dve", NdT[:C, :, :C], NT[:C, :, :C], M32[:C, :, :C], ALU.mult, reads=[NT, M32], writes=[NdT])
            P.tt("dve", d["C32T"][:C, :, :C], NT[:C, :, :C], Mc32[:C, :, :C], ALU.mult, reads=[NT, Mc32],
                 writes=[d["C32T"]])
            P.tt("pool", d["C64T"][:C, :, :C], NT[:C, :, :C], Mc64[:C, :, :C], ALU.mult, reads=[NT, Mc64],
                 writes=[d["C64T"]])
            P.copy("act", Zq[:C, :, :C], Z[:C, :, :C], reads=[Z], writes=[Zq])
            yield
            for lvl in range(1, 5):
                pNT = pp.next()
                for h in range(4):
                    P.mm(pNT[:C, h, :C], Nd[:C, h, :C], NdT[:C, h, :C], reads=[Nd, NdT], writes=[pNT])
                if lvl < 4:
                    pN = pp.next()
                    for h in range(4):
                        P.mm(pN[:C, h, :C], NdT[:C, h, :C], Nd[:C, h, :C], reads=[Nd, NdT], writes=[pN])
                NdT = d["NdT%d" % (lvl % 2)]
                P.copy("act", NdT[:C, :, :C], pNT[:C, :, :C], reads=[pNT], writes=[NdT])
                if lvl < 4:
                    Nd = d["Nd%d" % (lvl % 2)]
                    P.copy("dve", Nd[:C, :, :C], pN[:C, :, :C], reads=[pN], writes=[Nd])
                yield
                pZ = pp.next()
                for h in range(4):
                    P.mm(pZ[:C, h, :C], NdT[:C, h, :C], Zq[:C, h, :C], reads=[NdT, Zq], writes=[pZ])
                P.tt("dve", Z[:C, :, :C], Z[:C, :, :C], pZ[:C, :, :C], ALU.add, reads=[Z, pZ], writes=[Z])
                Zq = d["Zq%d" % (lvl % 2)] if lvl < 4 else d["X"]
                P.copy("act", Zq[:C, :, :C], Z[:C, :, :C], reads=[Z], writes=[Zq])
                yield
            X, XT = d["X"], d["XT"]
            pt_ = ptb.next()
            for h in range(4):
                P.tr(pt_[:C, h, :C], X[:C, h, :C], c.identb[:C, :C], reads=[X, c.identb], writes=[pt_])
            pU = pp.next()
            for h in range(4):
                P.mm(pU[:C, h, :C], d["C32T"][:C, h, :C], X[:C, h, :C], reads=[d["C32T"], X], writes=[pU])
            P.copy("act", XT[:C, :, :C], pt_[:C, 0:4, :C], reads=[pt_], writes=[XT])
            P.copy("dve", d["U"][:C, :, :C], pU[:C, :, :C], reads=[pU], writes=[d["U"]])
            yield
            pUp = pp.next()
            for h in range(4):
                P.mm(pUp[:C, h, :C], d["C32"][:C, h, :C], XT[:C, h, :C], reads=[d["C32"], XT], writes=[pUp])
            pW = pp.next()
            for h in range(4):
                P.mm(pW[:C, h, :C], XT[:C, h, :C], d["U"][:C, h, :C], reads=[XT, d["U"]], writes=[pW])
            P.copy("act", d["Up"][:C, :, :C], pUp[:C, :, :C], reads=[pUp], writes=[d["Up"]])
            P.tt("dve", d["X2"][:C, :, :C], X[:C, :, :C], pW[:C, :, :C], ALU.subtract, reads=[X, pW],
                 writes=[d["X2"]])
            yield
            pWp = pp.next()
            for h in range(4):
                P.mm(pWp[:C, h, :C], X[:C, h, :C], d["Up"][:C, h, :C], reads=[X, d["Up"]], writes=[pWp])
            pU2 = pp.next()
            for h in range(4):
                P.mm(pU2[:C, h, :C], d["C64T"][:C, h, :C], d["X2"][:C, h, :C], reads=[d["C64T"], d["X2"]],
                     writes=[pU2])
            P.tt("dve", d["XT2"][:C, :, :C], XT[:C, :, :C], pWp[:C, :, :C], ALU.subtract, reads=[XT, pWp],
                 writes=[d["XT2"]])
            P.copy("act", d["U"][:C, :, :C], pU2[:C, :, :C], reads=[pU2], writes=[d["U"]])
            yield
            pW2 = pp.next()
            for h in range(4):
                P.mm(pW2[:C, h, :C], d["XT2"][:C, h, :C], d["U"][:C, h, :C], reads=[d["XT2"], d["U"]], writes=[pW2])
            P.tt("dve", d["X3"][:C, :, :C], d["X2"][:C, :, :C], pW2[:C, :, :C], ALU.subtract, reads=[d["X2"], pW2],
                 writes=[d["X3"]])

        def scan(ci):
            c0, C = chunk_range(ci)
            d = slots[ci % NSLOT]
            kT, vt, zs, bg, s_, Qd, Kd, PT, Zq = (d["kT"], d["vt"], d["zs"], d["bg"], d["sm"], d["Qd"], d["Kd"],
                                                  d["PT"], d["X3"])
            pKS = pp.next()
            for h in range(4):
                P.mm(pKS[:C, h, :], kT[:, h, :C], Sb[:, h, :], reads=[kT, Sb], writes=[pKS])
            r_ = rb.next()
            for h in range(4):
                P.stt(r_[:C, h, :], pKS[:C, h, :], s_[:C, 8 + h:9 + h], vt[:C, h, :], ALU.mult, ALU.add,
                      reads=[pKS, s_, vt], writes=[r_])
            yield
            pVN = pp.next()
            for h in range(4):
                P.mm(pVN[:C, h, :], Zq[:C, h, :C], r_[:C, h, :], reads=[Zq, r_], writes=[pVN])
            v_ = vn.next()
            for h in range(4):
                if h < 2:
                    P.ts("dve", v_[:C, h, :], pVN[:C, h, :], bg[:C, h:h + 1], None, ALU.mult,
                         reads=[pVN, bg], writes=[v_])
                else:
                    P.act(v_[:C, h, :], pVN[:C, h, :], AF.Copy, reads=[pVN, bg], writes=[v_], scale=bg[:C, h:h + 1])
            yield
            po = pp.next()
            for h in range(4):
                P.mm(po[:, h, :C], Sb[:, h, :], Qd[:, h, :C], start=True, stop=False, reads=[Sb, Qd], writes=[po])
                P.mm(po[:, h, :C], v_[:C, h, :], PT[:C, h, :C], start=False, stop=True, reads=[v_, PT], writes=[po])
            pSN = pp.next()
            for h in range(4):
                P.mm(pSN[:, h, :], Kd[:C, h, :], v_[:C, h, :], reads=[Kd, v_], writes=[pSN])
            for h in range(4):
                P.stt(S[:, h, :], S[:, h, :], s_[:, 16 + h:17 + h], pSN[:, h, :], ALU.mult, ALU.add,
                      reads=[S, s_, pSN], writes=[S])
            P.copy("act", Sb[:], S[:], reads=[S], writes=[Sb])
            o_, q_, rs_, oo_ = osb.next(), osq.next(), ors.next(), oo.next()
            P.copy("act", o_[:, :, :C], po[:, :, :C], reads=[po], writes=[o_])
            P.act(q_[:, :, :C], po[:, :, :C], AF.Square, reads=[po], writes=[q_])
            yield
            pss = pp.next()
            for h in range(4):
                P.mm(pss[:, h, :C], c.onesb[:, :], q_[:, h, :C], reads=[c.onesb, q_], writes=[pss])
            P.act(rs_[:, :, :C], pss[:, :, :C], AF.Ln, reads=[pss], writes=[rs_], scale=1.0 / 128.0, bias=RMS_EPS)
            P.act(rs_[:, :, :C], rs_[:, :, :C], AF.Exp, reads=[rs_], writes=[rs_], scale=-0.5)
            P.tt("dve", o_[:, :, :C], o_[:, :, :C], rs_[:, :, :C], ALU.mult, reads=[o_, rs_], writes=[o_])
            P.stt(oo_[:, :, :C], o_[:, :, :C], normg[:, 0:1], zs[:, :, :C], ALU.mult, ALU.mult,
                  reads=[o_, normg, zs], writes=[oo_])
            P.dma(c.odnT[:, :, c0:c0 + C].rearrange("h p t -> p h t"), oo_[:, :, :C], reads=[oo_], writes=[Region()])
            yield

        def scan_seq(group):
            for ci in group:
                yield from scan(ci)

        def run_lockstep(gens):
            while gens:
                for g in list(gens):
                    try:
                        next(g)
                    except StopIteration:
                        gens.remove(g)

        groups = [list(range(g0, min(N_CH, g0 + GRP))) for g0 in range(0, N_CH, GRP)]
        def pre_item(ci):
            after = []
            if ci - GRP >= 0:
                after.append("pre%d" % (ci - GRP))
            if ci - NSLOT >= 0:
                after.append("scan%d" % (ci - NSLOT))
            return (None, (lambda _s, ci=ci: pre(ci)), "pre%d" % ci, tuple(after))

        def scan_item(ci):
            after = ["pre%d" % ci] + (["scan%d" % (ci - 1)] if ci > 0 else [])
            return (None, (lambda _s, ci=ci: scan(ci)), "scan%d" % ci, tuple(after))

        queue = [pre_item(ci) for ci in range(min(GRP, N_CH))]
        for ci in range(N_CH):
            queue.append(scan_item(ci))
            if ci + GRP < N_CH:
                queue.append(pre_item(ci + GRP))
        P.run_sched(queue)
        P.barrier()


def run_lockstep(gens):
    gens = list(gens)
    while gens:
        for g in list(gens):
            try:
                next(g)
            except StopIteration:
                gens.remove(g)


USE_SCHED = True


def phase3(c):
    P, nc = c.P, c.nc
    with ExitStack() as st:
        KT = sb(c, st, "KT", (128, 4, L), BF16)
        VA = sb(c, st, "VA", (128, 33, 512), BF16)
        VB = sb(c, st, "VB", (128, 33, 512), BF16)
        negTri = sb(c, st, "negTri", (128, 128), BF16)
        negOnes = sb(c, st, "negOnes", (128, 128), BF16)
        zer = sb(c, st, "zer", (128, 128), BF16)
        tmpm = sb(c, st, "tmpm", (128, 128), F32)
        QT = [sb(c, st, "QT%d" % i, (128, 4, 512), BF16) for i in range(2)]
        hs = []
        for i in range(8):
            hs.append({"e": sb(c, st, "e_%d" % i, (128, 512), F32), "sp": sb(c, st, "sp_%d" % i, (128, 512), BF16),
                       "sp2": sb(c, st, "sp2_%d" % i, (128, 512), BF16),
                       "W": sb(c, st, "W_%d" % i, (128, 512), BF16), "SP": sb(c, st, "SP_%d" % i, (128, 512), BF16),
                       "x": sb(c, st, "x_%d" % i, (128, 512), F32)})
        pos = [ps(c, st, "p3o_%d" % i, (128, 512), F32) for i in range(4)]
        ot = Rot([sb(c, st, "ot%d" % i, (128, 512), BF16) for i in range(3)])
        pz = Rot([ps(c, st, "p3z_%d" % i, (128, 512), F32) for i in range(2)])
        pa = Rot([ps(c, st, "p3a_%d" % i, (128, 512), F32) for i in range(2)])

        P.op("pool", lambda e: e.memset(tmpm[:], -1.0), writes=[tmpm])
        P.op("pool", lambda e: e.affine_select(out=tmpm[:], in_=tmpm[:], pattern=[[-1, 128]], compare_op=ALU.is_ge,
                                               fill=0.0, base=0, channel_multiplier=1), reads=[tmpm], writes=[tmpm])
        P.copy("pool", negTri[:], tmpm[:], reads=[tmpm], writes=[negTri])
        P.op("pool", lambda e: e.memset(negOnes[:], -1.0), writes=[negOnes])
        P.op("pool", lambda e: e.memset(zer[:], 0.0), writes=[zer])
        P.op("pool", lambda e: e.memset(tmpm[:], 1.0), reads=[negTri], writes=[tmpm])
        P.op("pool", lambda e: e.affine_select(out=tmpm[:], in_=tmpm[:], pattern=[[1, 128]], compare_op=ALU.is_gt,
                                               fill=0.0, base=0, channel_multiplier=-1), reads=[tmpm], writes=[tmpm])
        KT_r = [Region("KT%d" % i) for i in range(4)]
        for cc in range(4):
            P.dma(KT[:, cc, :], c.kT_sb[cc, :, :], writes=[KT_r[cc]])
        V_r = [Region("V%d" % i) for i in range(9)]
        Vall = Region("Vall")
        P.op("pool", lambda e: e.memset(VA[:], 0.0), writes=[Vall])
        P.dma(VB[:NMETA, 0, :], c.v_sb[0:NMETA, :], writes=[V_r[0]])
        for g in range(8):
            P.dma(VB[:, 1 + 4 * g:5 + 4 * g, :],
                  c.v_sb[NMETA + 512 * g:NMETA + 512 * (g + 1), :].rearrange("(b p) f -> p b f", p=128),
                  writes=[V_r[1 + g]])
        VAv = VA[:].rearrange("p b (c two d) -> p b c two d", two=2, d=64)
        VBv = VB[:].rearrange("p b (c two d) -> p b c two d", two=2, d=64)
        P.copy("dve", VAv[:NMETA, 0:1, :, 0, :], VBv[:NMETA, 0:1, :, 0, :], reads=V_r + [Vall], writes=[Vall])
        P.op("dve", lambda e: e.memset(VBv[:NMETA, 0:1, :, 0, :], 0.0), reads=[Vall], writes=[Vall])
        for g in range(8):
            eng = "dve" if g % 2 == 0 else "pool"
            blk = slice(1 + 4 * g, 5 + 4 * g)
            P.copy(eng, VAv[:, blk, :, 0, :], VBv[:, blk, :, 0, :], reads=V_r + [Vall], writes=[Vall])
            P.op(eng, lambda e, blk=blk: e.memset(VBv[:, blk, :, 0, :], 0.0), reads=[Vall], writes=[Vall])
        vreg = lambda kb: Vall
        done = {}

        def evac(j, h, po):
            t0, Tn = st_range(j)
            cc = h // 2
            done[(j, cc)] = done.get((j, cc), 0) + 1
            if done[(j, cc)] == 2:
                o = ot.next()
                P.copy("dve", o[:, :Tn], po[:, :Tn], reads=[po], writes=[o])
                P.dma(c.osbT[cc, :, t0:t0 + Tn], o[:, :Tn], reads=[o], writes=[Region()])

        def head_stream(h, delay):
            for _ in range(delay):
                yield
            cc, pb = h // 2, 64 * (h % 2)
            slot = hs[h]
            e_, W_, SP_, x_ = slot["e"], slot["W"], slot["SP"], slot["x"]
            sps = (slot["sp"], slot["sp2"])
            po = pos[cc]
            Vh = VA if h % 2 == 0 else VB
            for j in range(N_ST):
                t0, Tn = st_range(j)
                Q = QT[j % 2]
                if h == 0:
                    P.dma(Q[:, :, :Tn], c.qT_sb[:, :, t0:t0 + Tn].rearrange("c p t -> p c t"), writes=[Q])
                kbs = [0] if j == 0 else list(range(4 * j, -1, -1))
                P.op("dve", lambda e: e.memset(SP_[:], 0.0), writes=[SP_])
                if h % 2 == 1:
                    P.mm(po[:, :Tn], zer[:, :], Q[:, cc, :Tn], start=True, stop=False, reads=[zer, Q], writes=[po])
                for idx, kb in enumerate(kbs):
                    k0, KC = chunk_range(kb)
                    diag = (j == 0) or (kb >= 4 * j - 3)
                    q0 = 0 if j == 0 else (128 * (kb - (4 * j - 3)) if diag else 0)
                    N = Tn - q0
                    dw = min(128, N)
                    first, last = idx == 0, idx == len(kbs) - 1
                    sp_ = sps[idx % 2]
                    kt_ap = KT[pb:pb + 64, cc, k0:k0 + KC]
                    q_ap = Q[pb:pb + 64, cc, q0:q0 + N]
                    z = pz.next()
                    P.mm(z[:KC, :N], kt_ap, q_ap, reads=[KT_r[cc], Q], writes=[z])
                    P.act(e_[:KC, :N], z[:KC, :N], AF.Exp, reads=[z], writes=[e_])
                    if not first:
                        pKC, pq0, pN, psp = prev
                        P.tt("dve", SP_[:pKC, pq0:pq0 + pN], SP_[:pKC, pq0:pq0 + pN], psp[:pKC, :pN], ALU.add,
                             reads=[SP_, psp], writes=[SP_])
                    if diag:
                        P.tt("dve", e_[:KC, 0:dw], e_[:KC, 0:dw], tmpm[:KC, 0:dw], ALU.mult, reads=[e_, tmpm],
                             writes=[e_])
                    yield
                    P.act(sp_[:KC, :N], e_[:KC, :N], AF.Ln, reads=[e_], writes=[sp_], bias=1.0)
                    yield
                    a = pa.next()
                    P.mm(a[:KC, :N], negTri[:KC, :KC], sp_[:KC, :N], start=True, stop=first, reads=[negTri, sp_],
                         writes=[a])
                    if not first:
                        P.mm(a[:KC, :N], negOnes[:, :KC], SP_[:, q0:q0 + N], start=False, stop=True,
                             reads=[negOnes, SP_], writes=[a])
                    P.act(x_[:KC, :N], a[:KC, :N], AF.Exp, reads=[a], writes=[x_])
                    yield
                    P.tt("dve", W_[:KC, :N], x_[:KC, :N], e_[:KC, :N], ALU.mult, reads=[x_, e_], writes=[W_])
                    P.mm(po[:, q0:q0 + N], Vh[:KC, kb, cc * 128:(cc + 1) * 128], W_[:KC, :N], start=False,
                         stop=(last and h % 2 == 1),
                         reads=[vreg(kb), W_], writes=[po])
                    prev = (KC, q0, N, sp_)
                    yield
                evac(j, h, po)

        run_lockstep([head_stream(h, 0 if h < 4 else 2) for h in range(8)])
        P.barrier()


def layer_norm_tile(P, xt, rows, stt_, gbc, bbc, out):
    P.op("dve", lambda e: e.bn_stats(out=stt_[:rows, 0:6], in_=xt[:rows, 0:512]), reads=[xt], writes=[stt_])
    P.op("dve", lambda e: e.bn_stats(out=stt_[:rows, 6:12], in_=xt[:rows, 512:1024]), reads=[xt], writes=[stt_])
    P.op("dve", lambda e: e.bn_aggr(out=stt_[:rows, 12:14], in_=stt_[:rows, 0:12]), reads=[stt_], writes=[stt_])
    rsqrt(P, stt_[:rows, 14:15], stt_[:rows, 13:14], LN_EPS, [stt_], stt_)
    P.ts("dve", xt[:rows, :], xt[:rows, :], stt_[:rows, 12:13], stt_[:rows, 14:15], ALU.subtract, ALU.mult,
         reads=[xt, stt_], writes=[xt])
    P.tt("pool", xt[:rows, :], xt[:rows, :], gbc[:rows, :], ALU.mult, reads=[xt, gbc], writes=[xt])
    P.tt("pool", out[:rows, :], xt[:rows, :], bbc[:rows, :], ALU.add, reads=[xt, bbc], writes=[out])


def run_window2(queue, slotsets):
    free = {k: list(range(len(v))) for k, v in slotsets.items()}
    active = []
    queue = list(queue)
    while queue or active:
        while queue and queue[0] != "drain" and free[queue[0][0]]:
            cls, mk = queue.pop(0)
            si = free[cls].pop(0)
            active.append((mk(slotsets[cls][si]), cls, si))
        if queue and queue[0] == "drain" and not active:
            queue.pop(0)
            continue
        for item in list(active):
            try:
                next(item[0])
            except StopIteration:
                active.remove(item)
                free[item[1]].append(item[2])


def phase4(c):
    P, nc = c.P, c.nc
    with ExitStack() as st:
        Wbd = sb(c, st, "Wbd", (128, 4, D), BF16)
        Wbs = sb(c, st, "Wbs", (128, 4, D), BF16)
        Wout = sb(c, st, "Wout", (128, 8, D), BF16)
        gbc = sb(c, st, "g1bc", (128, D), F32)
        bbc = sb(c, st, "b1bc", (128, D), F32)
        Wr = sb(c, st, "Wr", (128, 8, 36), F32)
        rbb = sb(c, st, "rbb", (128, 36), F32)
        odn = [sb(c, st, "odn%d" % i, (128, 4, 512), BF16) for i in range(2)]
        osb_ = [sb(c, st, "osb4_%d" % i, (128, 4, 512), BF16) for i in range(2)]
        mT = [sb(c, st, "mT%d" % i, (128, 8, 512), BF16) for i in range(2)]
        mreg = [[Region("mT%d_%d" % (i, d_)) for d_ in range(8)] for i in range(2)]
        dcs = [{"ta": sb(c, st, "tA%d" % i, (128, 512), F32), "tb": sb(c, st, "tB%d" % i, (128, 512), F32),
                "g": sb(c, st, "gdc%d" % i, (128, 2, 512), BF16)} for i in range(5)]
        subs = [{"h0": sb(c, st, "h0b%d" % i, (128, D), F32), "h1": sb(c, st, "h1b%d" % i, (128, D), F32),
                 "st": sb(c, st, "st4_%d" % i, (128, 16), F32), "hf": sb(c, st, "hTf%d" % i, (128, 8, 128), F32),
                 "hb": sb(c, st, "hTb%d" % i, (128, 8, 128), BF16), "r": sb(c, st, "rt%d" % i, (128, 128), F32)}
                for i in range(4)]
        pAB = Rot([ps(c, st, "p4ab%d" % i, (128, 512), F32) for i in range(3)])
        pM = ps(c, st, "p4m", (128, D), F32)
        pT = ps(c, st, "p4T", (128, D), F32)
        pR = ps(c, st, "p4R", (128, 512), F32)

        for k in range(4):
            P.dma(Wbd[:, k, :], c.w_bdn[k * 128:(k + 1) * 128, :], writes=[Region()], eng="pool")
            P.dma(Wbs[:, k, :], c.w_bsb[k * 128:(k + 1) * 128, :], writes=[Region()], eng="pool")
        for k in range(8):
            P.dma(Wout[:, k, :], c.w_out[k * 128:(k + 1) * 128, :], writes=[Region()], eng="pool")
        P.dma(gbc[:], c.ln1_g.partition_broadcast(128), writes=[gbc])
        P.dma(bbc[:], c.ln1_b.partition_broadcast(128), writes=[bbc])
        P.dma(Wr[:, :, 0:4], c.rg_w.rearrange("(k p) g -> p k g", p=128), writes=[Region()])
        for g in range(NG):
            P.dma(Wr[:, :, 4 + 8 * g:12 + 8 * g], c.re_w[g].rearrange("(k p) e -> p k e", p=128), writes=[Region()])
        P.dma(rbb[:, 0:4], c.rg_b.partition_broadcast(128), writes=[Region()])
        P.dma(rbb[:, 4:36], c.re_b.rearrange("g e -> (g e)").partition_broadcast(128), writes=[Region()])
        P.barrier()

        def load_st(j):
            t0, Tn = st_range(j)
            a_, b_ = odn[j % 2], osb_[j % 2]
            P.dma(a_[:], c.odnT[:, :, t0:t0 + Tn].rearrange("c p t -> p c t"), writes=[a_])
            P.dma(b_[:], c.osbT[:, :, t0:t0 + Tn].rearrange("c p t -> p c t"), writes=[b_])

        def gen_dc(j, dc, slot):
            a_, b_, m_ = odn[j % 2], osb_[j % 2], mT[j % 2]
            t0, Tn = st_range(j)
            if dc == 0:
                load_st(j)
            ta, tb, g_ = slot["ta"], slot["tb"], slot["g"]
            P.dma(g_[:, 0, :], c.gsT[dc, :, t0:t0 + Tn], writes=[g_])
            P.dma(g_[:, 1, :], c.gsT[8 + dc, :, t0:t0 + Tn], writes=[g_])
            pa, pb = pAB.next(), pAB.next()
            for k in range(4):
                P.mm(pa[:], Wbd[:, k, dc * 128:(dc + 1) * 128], a_[:, k, :], start=(k == 0), stop=(k == 3),
                     reads=[a_], writes=[pa])
            for k in range(4):
                P.mm(pb[:], Wbs[:, k, dc * 128:(dc + 1) * 128], b_[:, k, :], start=(k == 0), stop=(k == 3),
                     reads=[b_], writes=[pb])
            P.tt("dve", ta[:], pa[:], g_[:, 0, :], ALU.mult, reads=[pa, g_], writes=[ta])
            P.tt("dve", tb[:], pb[:], g_[:, 1, :], ALU.mult, reads=[pb, g_], writes=[tb])
            yield
            P.tt("pool" if dc % 2 else "dve", m_[:, dc, :], ta[:], tb[:], ALU.add, reads=[ta, tb],
                 writes=[mreg[j % 2][dc]])
            yield

        def gen_sub(j, s_i, slot):
            t0, Tn = st_range(j)
            m_ = mT[j % 2]
            r0 = t0 + s_i * 128
            x0 = r0 - NMETA
            h0t, h1t, stt_, hf, hb_, r = slot["h0"], slot["h1"], slot["st"], slot["hf"], slot["hb"], slot["r"]
            P.dma(h0t[:], c.h0s[r0:r0 + 128, :], writes=[h0t])
            for half in range(2):
                for k in range(8):
                    P.mm(pM[:, half * 512:(half + 1) * 512], m_[:, k, s_i * 128:(s_i + 1) * 128],
                         Wout[:, k, half * 512:(half + 1) * 512], start=(k == 0), stop=(k == 7),
                         reads=[mreg[j % 2][k]], writes=[pM])
            P.stt(h0t[:], h0t[:], ALPHA, pM[:], ALU.mult, ALU.add, reads=[h0t, pM], writes=[h0t])
            yield
            P.op("dve", lambda e: e.bn_stats(out=stt_[:, 0:6], in_=h0t[:, 0:512]), reads=[h0t], writes=[stt_])
            P.op("dve", lambda e: e.bn_stats(out=stt_[:, 6:12], in_=h0t[:, 512:1024]), reads=[h0t], writes=[stt_])
            P.op("dve", lambda e: e.bn_aggr(out=stt_[:, 12:14], in_=stt_[:, 0:12]), reads=[stt_], writes=[stt_])
            P.act(stt_[:, 14:15], stt_[:, 13:14], AF.Ln, reads=[stt_], writes=[stt_], bias=LN_EPS)
            yield
            P.act(stt_[:, 14:15], stt_[:, 14:15], AF.Exp, reads=[stt_], writes=[stt_], scale=-0.5)
            P.stt(stt_[:, 15:16], stt_[:, 12:13], -1.0, stt_[:, 14:15], ALU.mult, ALU.mult, reads=[stt_],
                  writes=[stt_])
            yield
            P.act(h0t[:], h0t[:], AF.Identity, reads=[h0t, stt_], writes=[h0t], scale=stt_[:, 14:15],
                  bias=stt_[:, 15:16])
            yield
            P.tt("dve", h0t[:], h0t[:], gbc[:], ALU.mult, reads=[h0t, gbc], writes=[h0t])
            P.tt("pool", h1t[:], h0t[:], bbc[:], ALU.add, reads=[h0t, bbc], writes=[h1t])
            P.dma(c.h1s[x0:x0 + 128, :], h1t[:], reads=[h1t], writes=[Region()], eng="pool")
            yield
            for k in range(8):
                P.tr(pT[:, k * 128:(k + 1) * 128], h1t[:, k * 128:(k + 1) * 128], c.ident[:, :],
                     reads=[h1t, c.ident], writes=[pT])
            P.copy("act", hf[:], pT[:].rearrange("p (k t) -> p k t", k=8), reads=[pT], writes=[hf])
            yield
            P.copy("act", hb_[:], hf[:], reads=[hf], writes=[hb_])
            P.dma(c.h1T[:, :, x0:x0 + 128].rearrange("k p t -> p k t"), hb_[:], reads=[hb_], writes=[Region()], eng="pool")
            for k in range(8):
                P.mm(pR[:, 0:36], hf[:, k, :], Wr[:, k, :], start=(k == 0), stop=(k == 7), reads=[hf],
                     writes=[pR])
            P.tt("dve", r[:, 0:36], pR[:, 0:36], rbb[:, :], ALU.add, reads=[pR, rbb], writes=[r])
            yield
            yield from route(P, r)
            P.dma(c.combs[x0:x0 + 128, :], r[:, 96:128], reads=[r], writes=[Region()], eng="pool")

        def dc_item(j, dc):
            after = tuple("sub%d_%d" % (j - 2, s_i) for s_i in range(4)) if j - 2 >= 1 else ()
            return ("dc", (lambda sl_, j=j, dc=dc: gen_dc(j, dc, sl_)), "dc%d_%d" % (j, dc), after)

        def sub_item(j, s_i):
            return ("sub", (lambda sl_, j=j, s_i=s_i: gen_sub(j, s_i, sl_)), "sub%d_%d" % (j, s_i),
                    tuple("dc%d_%d" % (j, dc) for dc in range(8)))

        queue = [dc_item(1, dc) for dc in range(8)]
        for j in range(1, N_ST):
            a_items = [sub_item(j, s_i) for s_i in range(4)]
            b_items = [dc_item(j + 1, dc) for dc in range(8)] if j + 1 < N_ST else []
            while a_items or b_items:
                if a_items:
                    queue.append(a_items.pop(0))
                for _ in range(2):
                    if b_items:
                        queue.append(b_items.pop(0))
        P.run_sched(queue, {"dc": dcs, "sub": subs})
        P.barrier()


def route(P, r):
    R_, W_ = [r], [r]
    P.op("dve", lambda e: e.reduce_max(out=r[:, 36:37], in_=r[:, 0:4], axis=mybir.AxisListType.X), reads=R_, writes=W_)
    P.ts("dve", r[:, 37:38], r[:, 36:37], -1.0, None, ALU.mult, reads=R_, writes=W_)
    P.ts("dve", r[:, 43:47], r[:, 0:4], r[:, 36:37], None, ALU.is_equal, reads=R_, writes=W_)
    P.ts("dve", r[:, 48:56], r[:, 4:12], r[:, 43:44], None, ALU.mult, reads=R_, writes=W_)
    for g in range(1, NG):
        P.stt(r[:, 48:56], r[:, 4 + 8 * g:12 + 8 * g], r[:, 43 + g:44 + g], r[:, 48:56], ALU.mult, ALU.add,
              reads=R_, writes=W_)
    P.op("dve", lambda e: e.max(out=r[:, 56:64], in_=r[:, 48:56]), reads=R_, writes=W_)
    P.tt("dve", r[:, 64:65], r[:, 57:58], r[:, 56:57], ALU.subtract, reads=R_, writes=W_)
    yield
    P.act(r[:, 38:42], r[:, 0:4], AF.Exp, reads=R_, writes=W_, bias=r[:, 37:38])
    P.act(r[:, 64:65], r[:, 64:65], AF.Exp, reads=R_, writes=W_)
    yield
    P.op("dve", lambda e: e.reduce_sum(out=r[:, 42:43], in_=r[:, 38:42], axis=mybir.AxisListType.X), reads=R_,
         writes=W_)
    P.op("dve", lambda e: e.reciprocal(out=r[:, 42:43], in_=r[:, 42:43]), reads=R_, writes=W_)
    P.ts("dve", r[:, 65:66], r[:, 64:65], 1.0, None, ALU.add, reads=R_, writes=W_)
    P.op("dve", lambda e: e.reciprocal(out=r[:, 65:66], in_=r[:, 65:66]), reads=R_, writes=W_)
    P.tt("dve", r[:, 65:66], r[:, 65:66], r[:, 42:43], ALU.mult, reads=R_, writes=W_)
    P.tt("dve", r[:, 66:67], r[:, 65:66], r[:, 64:65], ALU.mult, reads=R_, writes=W_)
    P.ts("dve", r[:, 72:80], r[:, 48:56], r[:, 56:57], r[:, 65:66], ALU.is_equal, ALU.mult, reads=R_, writes=W_)
    P.ts("dve", r[:, 80:88], r[:, 48:56], r[:, 57:58], r[:, 66:67], ALU.is_equal, ALU.mult, reads=R_, writes=W_)
    P.tt("dve", r[:, 72:80], r[:, 72:80], r[:, 80:88], ALU.add, reads=R_, writes=W_)
    for g in range(NG):
        P.ts("dve", r[:, 96 + 8 * g:104 + 8 * g], r[:, 72:80], r[:, 43 + g:44 + g], None, ALU.mult, reads=R_,
             writes=W_)
    yield


def phase5(c):
    P, nc = c.P, c.nc
    NGRP = 4
    NEXP = NG * NE
    with ExitStack() as st:
        gbc = sb(c, st, "g2bc", (128, D), F32)
        bbc = sb(c, st, "b2bc", (128, D), F32)
        wgf = Rot([sb(c, st, "wgf%d" % i, (128, 8, FF), F32) for i in range(2)])
        wuf = Rot([sb(c, st, "wuf%d" % i, (128, 8, FF), F32) for i in range(2)])
        wdf = Rot([sb(c, st, "wdf%d" % i, (128, 2, D), F32) for i in range(2)])
        wgb = Rot([sb(c, st, "wgb%d" % i, (128, 8, FF), BF16) for i in range(2)])
        wub = Rot([sb(c, st, "wub%d" % i, (128, 8, FF), BF16) for i in range(2)])
        wdb = Rot([sb(c, st, "wdb%d" % i, (128, 2, D), BF16) for i in range(2)])
        hT = sb(c, st, "hT5", (128, 8, 1024), BF16)
        yaccs = [sb(c, st, "yacc%d" % i, (128, 8, D), F32) for i in range(2)]
        combs = [sb(c, st, "comb%d" % i, (128, 8, 32), F32) for i in range(2)]
        sg = Rot([sb(c, st, "sg%d" % i, (128, 512), F32) for i in range(2)])
        hid = Rot([sb(c, st, "hid%d" % i, (128, 2, 512), BF16) for i in range(2)])
        h1b = Rot([sb(c, st, "h1b5_%d" % i, (128, D), F32) for i in range(2)])
        ob = Rot([sb(c, st, "ob5_%d" % i, (128, D), F32) for i in range(2)])
        stats = Rot([sb(c, st, "st5_%d" % i, (128, 16), F32) for i in range(2)])
        pGU = Rot([ps(c, st, "p5gu%d" % i, (128, 512), F32) for i in range(4)])
        pY = Rot([ps(c, st, "p5y%d" % i, (128, D), F32) for i in range(2)])
        yreg = [[Region("yacc%d_%d" % (b_, i)) for i in range(8)] for b_ in range(2)]
        hreg = [Region("hT5_%d" % i) for i in range(2)]

        P.dma(gbc[:], c.ln2_g.partition_broadcast(128), writes=[gbc])
        P.dma(bbc[:], c.ln2_b.partition_broadcast(128), writes=[bbc])

        wts = {}

        def load_weights(grp, e_i):
            gf, uf, df = wgf.next(), wuf.next(), wdf.next()
            gb_, ub_, db_ = wgb.next(), wub.next(), wdb.next()
            P.dma(gf[:], c.e_gate[e_i].rearrange("(k p) f -> p k f", p=128), writes=[gf])
            P.dma(uf[:], c.e_up[e_i].rearrange("(k p) f -> p k f", p=128), writes=[uf])
            P.dma(df[:], c.e_down[e_i].rearrange("(k p) f -> p k f", p=128), writes=[df])
            P.copy("act", gb_[:], gf[:], reads=[gf], writes=[gb_])
            P.copy("pool", ub_[:], uf[:], reads=[uf], writes=[ub_])
            P.copy("act", db_[:, 0, :], df[:, 0, :], reads=[df], writes=[db_])
            P.copy("pool", db_[:, 1, :], df[:, 1, :], reads=[df], writes=[db_])
            wts[(grp, e_i)] = (gb_, ub_, db_)

        def gate_up(grp, e_i, half):
            gb_, ub_, db_ = wts[(grp, e_i)]
            hd = hid.next()
            for ffc in range(2):
                pg, pu = pGU.next(), pGU.next()
                for k in range(8):
                    P.mm(pg[:], gb_[:, k, ffc * 128:(ffc + 1) * 128], hT[:, k, half * 512:(half + 1) * 512],
                         start=(k == 0), stop=(k == 7), reads=[gb_, hreg[half]], writes=[pg])
                for k in range(8):
                    P.mm(pu[:], ub_[:, k, ffc * 128:(ffc + 1) * 128], hT[:, k, half * 512:(half + 1) * 512],
                         start=(k == 0), stop=(k == 7), reads=[ub_, hreg[half]], writes=[pu])
                s_ = sg.next()
                P.act(s_[:], pg[:], AF.Silu, reads=[pg], writes=[s_])
                P.tt("dve", hd[:, ffc, :], s_[:], pu[:], ALU.mult, reads=[s_, pu], writes=[hd])
            return hd

        def down(grp, e_i, half, hd):
            gb_, ub_, db_ = wts[(grp, e_i)]
            yacc, comb, yr = yaccs[grp % 2], combs[grp % 2], yreg[grp % 2]
            for s_i in range(4):
                ti = half * 4 + s_i
                py = pY.next()
                for hh in range(2):
                    for ffc in range(2):
                        P.mm(py[:, hh * 512:(hh + 1) * 512], hd[:, ffc, s_i * 128:(s_i + 1) * 128],
                             db_[:, ffc, hh * 512:(hh + 1) * 512], start=(ffc == 0), stop=(ffc == 1),
                             reads=[hd, db_], writes=[py])
                if e_i == 0:
                    P.ts("dve", yacc[:, ti, :], py[:], comb[:, ti, e_i:e_i + 1], None, ALU.mult,
                         reads=[py, comb], writes=[yr[ti]])
                else:
                    P.stt(yacc[:, ti, :], py[:], comb[:, ti, e_i:e_i + 1], yacc[:, ti, :], ALU.mult, ALU.add,
                          reads=[py, comb, yr[ti]], writes=[yr[ti]])

        def finish_tile(grp, ti):
            x0 = grp * 1024
            r0 = x0 + ti * 128
            yacc, yr = yaccs[grp % 2], yreg[grp % 2]
            h1t, o_, stt_ = h1b.next(), ob.next(), stats.next()
            P.dma(h1t[:], c.h1s[r0:r0 + 128, :], writes=[h1t])
            P.stt(h1t[:], h1t[:], ALPHA, yacc[:, ti, :], ALU.mult, ALU.add, reads=[h1t, yr[ti]], writes=[h1t])
            layer_norm_tile(P, h1t, 128, stt_, gbc, bbc, o_)
            P.dma(c.out[r0:r0 + 128, :], o_[:], reads=[o_], writes=[Region()])

        units = [(grp, e_i, half) for grp in range(NGRP) for e_i in range(NEXP) for half in range(2)]

        def prep_group(grp):
            x0 = grp * 1024
            for half in range(2):
                P.dma(hT[:, :, half * 512:(half + 1) * 512],
                      c.h1T[:, :, x0 + half * 512:x0 + (half + 1) * 512].rearrange("k p t -> p k t"),
                      writes=[hreg[half]])
            P.dma(combs[grp % 2][:], c.combs[x0:x0 + 1024, :].rearrange("(s p) e -> p s e", p=128),
                  writes=[combs[grp % 2]])

        prep_group(0)
        load_weights(0, 0)
        load_weights(0, 1)
        hd_prev = gate_up(*units[0])
        for ui, (grp, e_i, half) in enumerate(units):
            nxt = units[ui + 1] if ui + 1 < len(units) else None
            if nxt is not None:
                if nxt[2] == 0 and nxt[1] == 0:
                    prep_group(nxt[0])
                hd_next = gate_up(*nxt)
            down(grp, e_i, half, hd_prev)
            if nxt is not None:
                hd_prev = hd_next
                if nxt[2] == 0:
                    e2, g2 = nxt[1] + 1, nxt[0]
                    if e2 == NEXP:
                        e2, g2 = 0, g2 + 1
                    if g2 < NGRP:
                        load_weights(g2, e2)
            if grp > 0 and half == 1 and e_i < 8:
                finish_tile(grp - 1, e_i)
        for ti in range(8):
            finish_tile(NGRP - 1, ti)
        P.barrier()


_CACHE = {}


def _core_inputs(inputs, b):
    f32 = lambda a: np.ascontiguousarray(np.asarray(a, dtype=np.float32))
    m = {"x": f32(inputs["x"][b]), "meta_tokens": f32(inputs["meta_tokens"]),
         "ln_emb_g": f32(inputs["ln_emb_g"]), "ln_emb_b": f32(inputs["ln_emb_b"])}
    for n in ("w_in", "b_gate", "dn_conv_w", "dn_a_log", "dn_dt_bias", "dn_norm_g", "w_branch_dn", "w_branch_sb",
              "w_out", "ln1_g", "ln1_b", "router_group_w", "router_group_b", "router_expert_w", "router_expert_b",
              "ln2_g", "ln2_b"):
        m[n] = f32(np.asarray(inputs[n])[0])
    for n in ("expert_w_gate", "expert_w_up", "expert_w_down"):
        a = np.asarray(inputs[n])[0]
        m[n] = f32(a.reshape((NG * NE,) + a.shape[2:]))
    return m


def kernel(**inputs):
    if "nc" not in _CACHE:
        _CACHE["nc"] = build()[0]
    nc = _CACHE["nc"]
    nb = np.asarray(inputs["x"]).shape[0]
    shared = _core_inputs(inputs, 0)
    in_maps = []
    for b in range(nb):
        m = dict(shared)
        m["x"] = np.ascontiguousarray(np.asarray(inputs["x"][b], dtype=np.float32))
        in_maps.append(m)
    res = run_bass_kernel_spmd(nc, in_maps, core_ids=list(range(nb)))
    return np.stack([np.asarray(r["out"], dtype=np.float32) for r in res.results], axis=0)
```
